# Optimizing a Trainium2 kernel written in Bass

```python
import jax, jax.numpy as jnp
from jax import lax
import numpy as np

D_MODEL = 1024
BATCH = 2
SEQ = 16384
DEPTH = 1

CTX_LEN = 256
GRID_W = 64
EPS = 1e-6
N_MOD = 9
D_FF = 2816
M_HEADS = 4
M_HD = 128
M_W = M_HEADS * M_HD
CONV_W = 3
CHUNK = 128
POOL_WINDOWS = (2, 4, 8, 16)
POOL_GROUPS = len(POOL_WINDOWS)
POOL_W = 512
POOL_GW = POOL_W // POOL_GROUPS
MIX_W = M_W + POOL_W
M_COLS = 3 * M_W + 4 * M_HEADS
IN_COLS = M_COLS + POOL_W

kernel_name = 'hybrid_mlstm_pool_macaron_dit'


def rmsnorm(x, g):
    xf = x.astype(jnp.float32)
    y = xf * lax.rsqrt(jnp.mean(xf * xf, axis=-1, keepdims=True) + EPS)
    return (y * g.astype(jnp.float32)).astype(x.dtype)


def ada_in(x, mod, j, g):
    return rmsnorm(x, g) * (1 + mod[:, 3 * j + 1, None]) + mod[:, 3 * j, None]


def ada_out(x, z, mod, j, g, res_w):
    return x + res_w * mod[:, 3 * j + 2, None] * rmsnorm(z, g)


def swiglu(y, w_in, w_out):
    a, b = jnp.split(y @ w_in, 2, axis=-1)
    return (jax.nn.silu(a) * b) @ w_out


def centred_mean(u, win, axis):
    n = u.shape[axis]
    lo, hi = win // 2, win - 1 - win // 2
    pad = [(0, 0)] * u.ndim
    pad[axis] = (lo + 1, hi)
    s = jnp.cumsum(jnp.pad(u.astype(jnp.float32), pad), axis=axis)
    tot = lax.slice_in_dim(s, win, win + n, axis=axis) - lax.slice_in_dim(s, 0, n, axis=axis)
    t = jnp.arange(n)
    cnt = (jnp.minimum(t + hi, n - 1) - jnp.maximum(t - lo, 0) + 1).astype(jnp.float32)
    shape = [1] * u.ndim
    shape[axis] = n
    return (tot / cnt.reshape(shape)).astype(u.dtype)


def pool_mixer(u, w_pool, pool_scale, on_grid):
    bsz, n, _ = u.shape
    outs = []
    for gi, win in enumerate(POOL_WINDOWS):
        ug = u[..., gi * POOL_GW:(gi + 1) * POOL_GW]
        if on_grid:
            rows = n // GRID_W
            ug2 = ug.reshape(bsz, rows, GRID_W, POOL_GW)
            pg = centred_mean(centred_mean(ug2, win, 2), win, 1).reshape(bsz, n, POOL_GW)
        else:
            pg = centred_mean(ug, win, 1)
        outs.append(pg - ug)
    d = jnp.stack(outs, axis=2)
    y = jnp.einsum('bngc,gcd->bngd', d, w_pool).reshape(bsz, n, POOL_W)
    return y * pool_scale


def short_conv(u, w, b):
    n = u.shape[1]
    h = CONV_W // 2
    up = jnp.pad(u, ((0, 0), (h, h), (0, 0)))
    out = b
    for j in range(CONV_W):
        out = out + up[:, j:j + n] * w[j]
    return out


def mlstm_inputs(p, conv_w, conv_b, w_q, w_k, i_bias, f_bias):
    bsz, n, _ = p.shape
    u = jax.nn.silu(short_conv(p[..., :M_W], conv_w, conv_b)).reshape(bsz, n, M_HEADS, M_HD)
    q = jnp.einsum('bnhd,hde->bhne', u, w_q).astype(jnp.float32)
    k = jnp.einsum('bnhd,hde->bhne', u, w_k).astype(jnp.float32)
    v = p[..., M_W:2 * M_W].reshape(bsz, n, M_HEADS, M_HD).transpose(0, 2, 1, 3).astype(jnp.float32)
    g0 = 3 * M_W
    ig = (p[..., g0:g0 + 2 * M_HEADS].reshape(bsz, n, 2, M_HEADS) + i_bias).astype(jnp.float32)
    fg = (p[..., g0 + 2 * M_HEADS:M_COLS].reshape(bsz, n, 2, M_HEADS) + f_bias).astype(jnp.float32)
    return q, k, v, ig.transpose(2, 0, 3, 1), fg.transpose(2, 0, 3, 1)


def mlstm_chunkwise(q, k, v, i_pre, f_pre):
    bsz, nh, t_len, dk = q.shape
    nc = t_len // CHUNK
    q = q.reshape(bsz, nh, nc, CHUNK, dk)
    k = k.reshape(bsz, nh, nc, CHUNK, dk) * (dk ** -0.5)
    v = v.reshape(bsz, nh, nc, CHUNK, dk)
    ip = i_pre.reshape(bsz, nh, nc, CHUNK)
    b = jnp.cumsum(jax.nn.log_sigmoid(f_pre).reshape(bsz, nh, nc, CHUNK), axis=-1)
    g = b[..., -1]
    a = g[..., None] - b + ip
    m_loc = jnp.max(a, axis=-1)
    wk = jnp.exp(a - m_loc[..., None])[..., None] * k
    ckv = jnp.einsum('bhclk,bhclv->bhckv', wk, v)
    cn = jnp.sum(wk, axis=3)

    def step(carry, inp):
        c_st, n_st, m_st = carry
        g_c, m_c, ckv_c, cn_c = inp
        m_new = jnp.maximum(g_c + m_st, m_c)
        a_old = jnp.exp(g_c + m_st - m_new)
        a_new = jnp.exp(m_c - m_new)
        c_new = a_old[..., None, None] * c_st + a_new[..., None, None] * ckv_c
        n_new = a_old[..., None] * n_st + a_new[..., None] * cn_c
        return (c_new, n_new, m_new), (c_st, n_st, m_st)

    init = (jnp.zeros((bsz, nh, dk, dk), jnp.float32), jnp.zeros((bsz, nh, dk), jnp.float32),
            jnp.zeros((bsz, nh), jnp.float32))
    xs = (jnp.moveaxis(g, -1, 0), jnp.moveaxis(m_loc, -1, 0), jnp.moveaxis(ckv, 2, 0), jnp.moveaxis(cn, 2, 0))
    _, (c_in, n_in, m_in) = lax.scan(step, init, xs)
    c_in = jnp.moveaxis(c_in, 0, 2)
    n_in = jnp.moveaxis(n_in, 0, 2)
    m_in = jnp.moveaxis(m_in, 0, -1)
    mask = jnp.tril(jnp.ones((CHUNK, CHUNK), dtype=bool))
    dmat = jnp.where(mask, b[..., :, None] - b[..., None, :] + ip[..., None, :], -jnp.inf)
    inter = b + m_in[..., None]
    m_t = jnp.maximum(jnp.max(dmat, axis=-1), inter)
    s = jnp.einsum('bhctk,bhcsk->bhcts', q, k) * jnp.exp(dmat - m_t[..., None])
    w_inter = jnp.exp(inter - m_t)
    num = w_inter[..., None] * jnp.einsum('bhctk,bhckv->bhctv', q, c_in) + jnp.einsum('bhcts,bhcsv->bhctv', s, v)
    den = w_inter * jnp.einsum('bhctk,bhck->bhct', q, n_in) + jnp.sum(s, axis=-1)
    h = num / jnp.maximum(jnp.abs(den), jnp.exp(-m_t))[..., None]
    return h.reshape(bsz, nh, t_len, dk)


def mlstm_bidir(p_lat, p_ctx, conv_w, conv_b, w_q, w_k, i_bias, f_bias):
    ql, kl, vl, il, fl = mlstm_inputs(p_lat, conv_w, conv_b, w_q, w_k, i_bias, f_bias)
    qc, kc, vc, ic, fc = mlstm_inputs(p_ctx, conv_w, conv_b, w_q, w_k, i_bias, f_bias)
    lc = qc.shape[2]
    cat = lambda a, b: jnp.concatenate([a, b], axis=2)
    rev = lambda a: jnp.flip(a, axis=2)
    h_f = mlstm_chunkwise(cat(qc, ql), cat(kc, kl), cat(vc, vl), cat(ic[0], il[0]), cat(fc[0], fl[0]))
    h_b = mlstm_chunkwise(cat(rev(qc), rev(ql)), cat(rev(kc), rev(kl)), cat(rev(vc), rev(vl)),
                          cat(rev(ic[1]), rev(il[1])), cat(rev(fc[1]), rev(fl[1])))
    h_lat = h_f[:, :, lc:] + rev(h_b[:, :, lc:])
    h_ctx = h_f[:, :, :lc] + rev(h_b[:, :, :lc])
    return h_lat, h_ctx


def mlstm_out(h, o, g):
    bsz, _, n, _ = h.shape
    h = h.transpose(0, 2, 1, 3)
    h = h * lax.rsqrt(jnp.mean(h * h, axis=-1, keepdims=True) + EPS) * g.reshape(M_HEADS, M_HD).astype(jnp.float32)
    return (h.reshape(bsz, n, M_W) * jax.nn.sigmoid(o.astype(jnp.float32))).astype(o.dtype)


def setup_inputs(seed: int = 0) -> dict:
    key = jax.random.key(seed)
    ks = jax.random.split(key, 24)
    nrm = lambda k, shape, s: jax.random.normal(k, shape, jnp.float32) * s
    f_base = jnp.linspace(3.0, 6.0, M_HEADS, dtype=jnp.float32)
    return {
        'x': nrm(ks[0], (BATCH, SEQ, D_MODEL), 1.0),
        'c': nrm(ks[1], (BATCH, D_MODEL), 1.0),
        'ctx': nrm(ks[2], (BATCH, CTX_LEN, D_MODEL), 1.0),
        'c_ctx': nrm(ks[3], (D_MODEL,), 1.0),
        'w_mod': nrm(ks[4], (DEPTH, D_MODEL, N_MOD * D_MODEL), D_MODEL ** -0.5),
        'b_mod': nrm(ks[5], (DEPTH, N_MOD * D_MODEL), 0.02),
        'norm_pre': 1.0 + nrm(ks[6], (DEPTH, 3, D_MODEL), 0.05),
        'norm_post': 1.0 + nrm(ks[7], (DEPTH, 3, D_MODEL), 0.05),
        'ffn_w_in': nrm(ks[8], (DEPTH, 2, D_MODEL, 2 * D_FF), D_MODEL ** -0.5),
        'ffn_w_out': nrm(ks[9], (DEPTH, 2, D_FF, D_MODEL), D_FF ** -0.5),
        'w_in': nrm(ks[10], (DEPTH, D_MODEL, IN_COLS), D_MODEL ** -0.5),
        'w_out': nrm(ks[11], (DEPTH, MIX_W, D_MODEL), MIX_W ** -0.5),
        'conv_w': nrm(ks[12], (DEPTH, CONV_W, M_W), CONV_W ** -0.5),
        'conv_b': nrm(ks[13], (DEPTH, M_W), 0.02),
        'w_q': nrm(ks[14], (DEPTH, M_HEADS, M_HD, M_HD), M_HD ** -0.5),
        'w_k': nrm(ks[15], (DEPTH, M_HEADS, M_HD, M_HD), M_HD ** -0.5),
        'i_bias': nrm(ks[16], (DEPTH, 2, M_HEADS), 0.1),
        'f_bias': f_base + nrm(ks[17], (DEPTH, 2, M_HEADS), 0.1),
        'head_norm': 1.0 + nrm(ks[18], (DEPTH, M_W), 0.05),
        'pool_w': nrm(ks[19], (DEPTH, POOL_GROUPS, POOL_GW, POOL_GW), POOL_GW ** -0.5),
        'pool_scale': 1.0 + nrm(ks[20], (DEPTH, POOL_W), 0.1),
    }


def reference(x, c, ctx, c_ctx, w_mod, b_mod, norm_pre, norm_post, ffn_w_in, ffn_w_out, w_in, w_out,
              conv_w, conv_b, w_q, w_k, i_bias, f_bias, head_norm, pool_w, pool_scale):
    sc = jax.nn.silu(c)
    scc = jax.nn.silu(c_ctx)[None]
    xc = ctx
    for l in range(DEPTH):
        ctx_out = l < DEPTH - 1
        mod = (sc @ w_mod[l] + b_mod[l]).reshape(-1, N_MOD, D_MODEL)
        modc = (scc @ w_mod[l] + b_mod[l]).reshape(1, N_MOD, D_MODEL)
        x = ada_out(x, swiglu(ada_in(x, mod, 0, norm_pre[l, 0]), ffn_w_in[l, 0], ffn_w_out[l, 0]),
                    mod, 0, norm_post[l, 0], 0.5)
        xc = ada_out(xc, swiglu(ada_in(xc, modc, 0, norm_pre[l, 0]), ffn_w_in[l, 0], ffn_w_out[l, 0]),
                     modc, 0, norm_post[l, 0], 0.5)
        p = ada_in(x, mod, 1, norm_pre[l, 1]) @ w_in[l]
        pc = ada_in(xc, modc, 1, norm_pre[l, 1]) @ (w_in[l] if ctx_out else w_in[l, :, :M_COLS])
        h_lat, h_ctx = mlstm_bidir(p[..., :M_COLS], pc[..., :M_COLS], conv_w[l], conv_b[l],
                                   w_q[l], w_k[l], i_bias[l], f_bias[l])
        mix = jnp.concatenate([mlstm_out(h_lat, p[..., 2 * M_W:3 * M_W], head_norm[l]),
                               pool_mixer(p[..., M_COLS:], pool_w[l], pool_scale[l], True)], axis=-1)
        x = ada_out(x, mix @ w_out[l], mod, 1, norm_post[l, 1], 1.0)
        if ctx_out:
            mixc = jnp.concatenate([mlstm_out(h_ctx, pc[..., 2 * M_W:3 * M_W], head_norm[l]),
                                    pool_mixer(pc[..., M_COLS:], pool_w[l], pool_scale[l], False)], axis=-1)
            xc = ada_out(xc, mixc @ w_out[l], modc, 1, norm_post[l, 1], 1.0)
        x = ada_out(x, swiglu(ada_in(x, mod, 2, norm_pre[l, 2]), ffn_w_in[l, 1], ffn_w_out[l, 1]),
                    mod, 2, norm_post[l, 2], 0.5)
        if ctx_out:
            xc = ada_out(xc, swiglu(ada_in(xc, modc, 2, norm_pre[l, 2]), ffn_w_in[l, 1], ffn_w_out[l, 1]),
                         modc, 2, norm_post[l, 2], 0.5)
    return x
```

```python
import os
import numpy as np
from contextlib import ExitStack
import concourse.bass as bass
import concourse.mybir as mybir
from concourse.bass_utils import run_bass_kernel_spmd

F32 = mybir.dt.float32
BF16 = mybir.dt.bfloat16
AF = mybir.ActivationFunctionType
ALU = mybir.AluOpType
AX = mybir.AxisListType
SEM_CAP = 20000
KSTOP = float(os.environ.get('KSTOP', '99'))
KOFF = os.environ.get('KOFF', '').split(',')
ACCT_ONLY = False
NCORES = 8


class Buf:
    __slots__ = ("w", "r")

    def __init__(self):
        self.w = None
        self.r = []


class DGroup:
    __slots__ = ("key", "final")

    def __init__(self, key):
        self.key = key
        self.final = 0


class Op:
    __slots__ = ("eng", "fn", "deps", "signal", "sig", "grp", "dval", "isdma")

    def __init__(self, eng, fn):
        self.eng = eng
        self.fn = fn
        self.deps = []
        self.signal = False
        self.sig = 0
        self.grp = None
        self.dval = 0
        self.isdma = False


class Prog:
    ENGS = ("pe", "act", "dve", "pool", "sp")

    def __init__(self, nc):
        self.nc = nc
        self.ops = {e: [] for e in self.ENGS}
        self.dcount = {}
        self.dlast = {}

    def _deps(self, op, reads, writes):
        deps = []
        for b in reads:
            if b.w is not None:
                deps.append(("raw", b.w))
            b.r.append(op)
        for b in writes:
            if b.w is not None:
                deps.append(("waw", b.w))
            for r in b.r:
                if r is not op:
                    deps.append(("war", r))
            b.w = op
            b.r = []
        out = []
        seen = set()
        for kind, d in deps:
            if d is op or id(d) in seen:
                continue
            if (not d.isdma) and (not op.isdma) and d.eng == op.eng:
                if op.eng == "pe":
                    continue
            seen.add(id(d))
            out.append(d)
        op.deps = out
        for d in out:
            d.signal = True

    def op(self, eng, fn, reads=(), writes=()):
        o = Op(eng, fn)
        self._deps(o, list(reads), list(writes))
        self.ops[eng].append(o)
        return o

    def dma(self, eng, out, in_, reads=(), writes=(), sem=None, join=False, **kw):
        o = Op(eng, lambda e: e.dma_start(out=out, in_=in_, **kw))
        o.isdma = True
        key = sem
        if join and key in self.dlast:
            group = self.dlast[key]
            prev = None
        else:
            group = DGroup(key)
            prev = self.dlast.get(key)
            self.dlast[key] = group
        o.grp = group
        self._deps(o, list(reads), list(writes))
        o.deps = [d for d in o.deps if not (isinstance(d, Op) and d.grp is group and d is not o)]
        if prev is not None:
            o.deps.append(prev)
        self.dcount[key] = self.dcount.get(key, 0) + 16
        group.final = self.dcount[key]
        o.signal = True
        self.ops[eng].append(o)
        return o

    def cc(self, fn, reads=(), writes=(), sem=None):
        o = Op("pool", fn)
        o.isdma = True
        group = DGroup(sem)
        assert sem not in self.dcount
        self.dcount[sem] = 1
        group.final = 1
        self.dlast[sem] = group
        o.grp = group
        self._deps(o, list(reads), list(writes))
        o.signal = True
        o.dval = -1
        self.ops["pool"].append(o)
        return o

    def barrier(self, tiny):
        a = {}
        for e in ("pe", "act", "dve", "pool"):
            o = Op(e, tiny[e])
            o.signal = True
            self.ops[e].append(o)
            a[e] = o
        groups = [g for g in self.dlast.values()]
        for e in self.ENGS:
            o = Op(e, None)
            o.deps = [a[x] for x in a if x != e] + groups
            self.ops[e].append(o)

    def emit(self):
        nc = self.nc
        for e in self.ENGS:
            n = 0
            for o in self.ops[e]:
                if o.isdma:
                    continue
                if o.signal:
                    n += 1
                    o.sig = n
        nsem = {e: max(1, -(-max([o.sig for o in self.ops[e] if not o.isdma] + [0]) // SEM_CAP)) for e in self.ENGS}
        with ExitStack() as st:
            esems = {e: [st.enter_context(nc.semaphore(f"s_{e}{i}")) for i in range(nsem[e])] for e in self.ENGS}
            dsems = {k: st.enter_context(nc.semaphore(f"d_{i}")) for i, k in enumerate(self.dcount)}
            block = st.enter_context(nc.Block())

            def run(ename, eng):
                waited = {}
                for o in self.ops[ename]:
                    for d in o.deps:
                        if isinstance(d, DGroup):
                            s, v = dsems[d.key], d.final
                        elif d.isdma:
                            s, v = dsems[d.grp.key], d.grp.final
                        else:
                            k = (d.sig - 1) // SEM_CAP
                            s, v = esems[d.eng][k], d.sig - k * SEM_CAP
                        sid = id(s)
                        if waited.get(sid, 0) >= v:
                            continue
                        waited[sid] = v
                        eng.wait_ge(s, v)
                    if o.fn is None:
                        continue
                    ins = o.fn(eng)
                    if o.isdma and o.dval == -1:
                        ins.then_inc(dsems[o.grp.key])
                    elif o.isdma:
                        ins.then_inc(dsems[o.grp.key], 16)
                    elif o.signal:
                        k = (o.sig - 1) // SEM_CAP
                        ins.then_inc(esems[ename][k], 1)

            @block.tensor
            def _(e):
                run("pe", e)

            @block.scalar
            def _(e):
                run("act", e)

            @block.vector
            def _(e):
                run("dve", e)

            @block.gpsimd
            def _(e):
                run("pool", e)

            @block.sync
            def _(e):
                run("sp", e)


class Cfg:
    def __init__(self, D=1024, DFF=2816, NT=8, CTX=256):
        self.D, self.DFF, self.NT, self.CTX = D, DFF, NT, CTX
        self.KC, self.JC = D // 128, DFF // 128
        self.CC = CTX // 128
        self.NA = CTX + 128
        self.NB = NT * 4
        self.INC = 2064
        self.SEQ = 4 * NT * 512
        self.ROWS = self.SEQ // 64


POOL_WINDOWS = (2, 4, 8, 16)


def pool_offsets():
    offs = []
    for g, w in enumerate(POOL_WINDOWS):
        lo, hi = w // 2, w - 1 - w // 2
        for dl in range(-((lo + 1) // 2), (1 + hi) // 2 + 1):
            offs.append((g, dl))
    return offs


def host_tables(cfg, core):
    b, r = core // 4, core % 4
    t = {}
    fl = np.array([r == 0, r == 3, r > 0, r < 3], np.float32)
    t["flags"] = np.tile(fl[None, :], (128, 1)).astype(np.float32)
    pred = np.zeros((2, 8), np.float32)
    mbt = np.zeros((2, 8, 8), np.float32)
    for q in range(8):
        if q // 4 != b:
            continue
        if q < core:
            pred[0, q] = 1
            for q2 in range(q + 1, core):
                mbt[0, q, q2] = 1
        if q > core:
            pred[1, q] = 1
            for q2 in range(core + 1, q):
                mbt[1, q, q2] = 1
    predt = np.zeros((8, 8), np.float32)
    for q in range(8):
        predt[q, 0:4] = pred[0, q]
        predt[q, 4:8] = pred[1, q]
    t["pred"] = np.tile(predt.reshape(1, 64), (128, 1)).astype(np.float32)
    t["mbt"] = np.tile(mbt.reshape(1, 128), (128, 1)).astype(np.float32)
    sel = np.zeros((2, 8), np.float32)
    if r > 0:
        sel[0, core - 1] = 1
    if r < 3:
        sel[1, core + 1] = 1
    t["sel"] = np.tile(sel.reshape(1, 16), (128, 1)).astype(np.float32)
    R = cfg.ROWS
    inv = np.zeros((128, cfg.NB, 4), np.float32)
    for blk in range(cfg.NB):
        for p in range(128):
            row = r * cfg.NT * 8 + blk * 2 + p // 64
            col = p % 64
            for g, w in enumerate(POOL_WINDOWS):
                lo, hi = w // 2, w - 1 - w // 2
                cr = min(row + hi, R - 1) - max(row - lo, 0) + 1
                cc = min(col + hi, 63) - max(col - lo, 0) + 1
                inv[p, blk, g] = 1.0 / (cr * cc)
    t["invcnt"] = inv.reshape(128, cfg.NB * 4)
    offs = pool_offsets()
    pm = np.zeros((128, len(offs), 128), np.float32)
    tin = np.arange(128)
    for i, (g, dl) in enumerate(offs):
        w = POOL_WINDOWS[g]
        lo, hi = w // 2, w - 1 - w // 2
        rin = 2 * dl + tin // 64
        cin = tin % 64
        for to in range(128):
            ro, co = to // 64, to % 64
            ok = (rin >= ro - lo) & (rin <= ro + hi) & (cin >= co - lo) & (cin <= co + hi)
            pm[:, i, to] = ok.astype(np.float32)
    t["pm"] = pm.reshape(128, len(offs) * 128)
    return t


class T:
    def __init__(self, t, nb=1):
        self.t = t
        self.b = [Buf() for _ in range(nb)]

    @property
    def B(self):
        return self.b


def build(cfg):
    D, DFF, NT, KC, JC, CC, NA, NB = cfg.D, cfg.DFF, cfg.NT, cfg.KC, cfg.JC, cfg.CC, cfg.NA, cfg.NB
    EPS = 1e-6
    nc = bass.Bass("TRN2", target_bir_lowering=False)
    P = Prog(nc)
    offs = pool_offsets()
    NOFF = len(offs)

    def din(name, shape, dt=F32):
        return nc.dram_tensor(name, list(shape), dt, kind="ExternalInput")

    x_loc = din("x_loc", [NT * 512, D])
    x_aux = din("x_aux", [NA, D])
    c_loc = din("c_loc", [D])
    c_ctx = din("c_ctx", [D])
    w_mod = din("w_mod", [D, 9 * D])
    b_mod = din("b_mod", [9 * D])
    norm_pre = din("norm_pre", [3 * D])
    norm_post = din("norm_post", [3 * D])
    ffn_w_in = din("ffn_w_in", [2, D, 2 * DFF])
    ffn_w_out = din("ffn_w_out", [2, DFF, D])
    w_in = din("w_in", [D, cfg.INC])
    w_out = din("w_out", [1024, D])
    conv_w = din("conv_w", [3 * 512])
    conv_b = din("conv_b", [512])
    w_q = din("w_q", [4, 128, 128])
    w_k = din("w_k", [4, 128, 128])
    gbias = din("gbias", [16])
    head_norm = din("head_norm", [512])
    pool_w = din("pool_w", [4, 128, 128])
    pool_scale = din("pool_scale", [512])
    flags_d = din("flags", [128, 4])
    pred_d = din("pred", [128, 64])
    mbt_d = din("mbt", [128, 128])
    sel_d = din("sel", [128, 16])
    invcnt_d = din("invcnt", [128, NB * 4])
    pm_d = din("pm", [128, NOFF * 128])
    out_d = nc.dram_tensor("out", [NT * 512, D], F32, kind="ExternalOutput")

    Win_s = nc.dram_tensor("Win_s", [2 * JC, 128, KC * 256], BF16)
    Wout_s = nc.dram_tensor("Wout_s", [2 * KC, 128, JC * 128], BF16)
    Wi_s = nc.dram_tensor("Wi_s", [128, KC * cfg.INC], BF16)
    Wo_s = nc.dram_tensor("Wo_s", [128, 8 * D], BF16)
    X1_s = nc.dram_tensor("X1_s", [NT, 128, KC * 512], F32)
    QT_s = nc.dram_tensor("QT_s", [NT, 128, 2048], BF16)
    KT_s = nc.dram_tensor("KT_s", [NT, 128, 2048], BF16)
    KTOK_s = nc.dram_tensor("KTOK_s", [NB, 128, 512], BF16)
    V_s = nc.dram_tensor("V_s", [NB, 128, 4 * 129], BF16)
    O_s = nc.dram_tensor("O_s", [NB, 128, 512], F32)
    PL_s = nc.dram_tensor("PL_s", [NB, 128, 512], F32)
    PLb_s = nc.dram_tensor("PLb_s", [NB, 128, 512], BF16)
    SNAP_s = nc.dram_tensor("SNAP_s", [NB, 128, 4 * 129], BF16)
    CCW = 1040
    cc1_in = nc.dram_tensor("cc1_in", [128, CCW], F32)
    cc1_out = nc.dram_tensor("cc1_out", [128 * NCORES, CCW], F32)
    cc2_in = nc.dram_tensor("cc2_in", [128, 4096], BF16)
    cc2_out = nc.dram_tensor("cc2_out", [128 * NCORES, 4096], BF16)
    dB = {k: [Buf() for _ in range(n)] for k, n in dict(Win=2 * JC, Wout=2 * KC, Wi=1, Wo=1, X1=NT, QT=NT, KT=NT,
                                                         KTOK=NB, V=NB, O=NB, PL=NB, PLb=NB, SNAP=NB, cc1i=1, cc1o=1,
                                                         cc2i=1, cc2o=1, out=1).items()}

    st = ExitStack()
    with st:
        acct = {}

        def sb(name, shape, dt=F32, nb=1):
            n = 1
            for v in shape[1:]:
                n *= v
            acct[name] = -(-(n * (2 if dt == BF16 else 4)) // 32) * 32
            if ACCT_ONLY:
                return T(None, nb)
            return T(st.enter_context(nc.sbuf_tensor(name, list(shape), dt)), nb)

        def al(base, off, shape, dt, nb=None):
            n = 1
            for v in shape[1:]:
                n *= v
            nbytes = n * (2 if dt == BF16 else 4)
            t = T(None, 0)
            t.b = list(base.b) if nb is None else [base.b[0]] * nb
            if ACCT_ONLY:
                return t
            bt = base.t
            flat = bt[:] if len(bt.shape) == 2 else (bt[:].rearrange("p a b -> p (a b)") if len(bt.shape) == 3 else
                                                     bt[:].rearrange("p a b c -> p (a b c)"))
            esz = 2 if flat.dtype == BF16 else 4
            assert off % 32 == 0 and off % esz == 0 and (off + nbytes) <= flat.shape[1] * esz, (off, nbytes, flat.shape)
            v = flat[:, off // esz:(off + nbytes + esz - 1) // esz]
            if flat.dtype != dt:
                v = v.bitcast(dt)
            v = v[:, 0:n]
            if len(shape) == 3:
                v = v.rearrange("p (a b) -> p a b", a=shape[1])
            elif len(shape) == 4:
                v = v.rearrange("p (a b c) -> p a b c", a=shape[1], b=shape[2])
            t.t = v
            return t

        identF = sb("identF", [128, 128]); identB = sb("identB", [128, 128], BF16)
        onesF = sb("onesF", [128, 128]); onesB = sb("onesB", [128, 128], BF16)
        triF = sb("triF", [128, 128]); triB = sb("triB", [128, 128])
        tiny = sb("tiny", [128, 8], F32, nb=4)
        NV = 2 * KC + 9 * KC + 3 * KC + 3 * KC + 12 + 4 + 4
        VT = sb("VT", [128, NV])
        SC = sb("SC", [128, KC, 2])
        MODT = sb("MODT", [128, 9 * KC, 2])
        AIN = sb("AIN", [128, 3, KC, 2]); AOUT = sb("AOUT", [128, 3, KC, 2])
        flags = sb("flags_sb", [128, 4]); predt = sb("pred_sb", [128, 8, 8]); mbt = sb("mbt_sb", [128, 2, 8, 8])
        sel = sb("sel_sb", [128, 2, 8]); invcnt = sb("invcnt_sb", [128, NB, 4])
        PM = sb("PM", [128, NOFF, 128], BF16)
        HNORM = sb("HNORM", [128, 512]); GBIAS = sb("GBIAS", [128, 16])
        wq = sb("wq", [128, 4, 128], BF16); wk = sb("wk", [128, 4, 128], BF16); pw = sb("pw", [128, 4, 128], BF16)
        BIG0 = sb("BIG0", [128, 4096], F32, nb=8)
        BIG1 = sb("BIG1", [128, 4096], F32, nb=8)
        yT = sb("yT", [128, KC, 512], BF16, nb=KC)
        hT = sb("hT", [128, max(JC * 512, 11264)], BF16, nb=JC)
        sq = [sb(f"sq{i}", [128, 512], BF16) for i in range(2)]
        rstd = sb("rstd", [128, 512])
        tmpf = [sb(f"tmpf{i}", [128, 512]) for i in range(3)]
        WIN = [sb(f"WIN{i}", [128, KC, 256], BF16) for i in range(3)]
        WOUT = [sb(f"WOUT{i}", [128, JC, 128], BF16) for i in range(2)]
        XIO = [sb(f"XIO{i}", [128, D]) for i in range(2)]
        rowsA = al(XIO[0], 0, [128, 128], F32); rowsB = al(XIO[1], 0, [128, 128], F32)
        QT = sb("QT", [128, 4, 512], BF16); KT = sb("KT", [128, 4, 512], BF16)
        KTOK = [sb(f"KTOK{i}", [128, 4, 128], BF16) for i in range(2)]
        VXT = [sb(f"VXT{i}", [128, 4, 129], BF16) for i in range(8)]
        OT = [sb(f"OT{i}", [128, 512]) for i in range(2)]
        PT = [sb(f"PT{i}", [128, 512]) for i in range(2)]
        PTb = [sb(f"PTb{i}", [128, 512], BF16) for i in range(2)]
        VW = [sb(f"VW{i}", [128, 4, 129], BF16) for i in range(2)]
        NCH = NB + CC
        GP = sb("GP", [128, NCH, 32], F32, nb=NCH)
        GPX = sb("GPX", [128, NCH, 4], F32, nb=NCH)
        GA = sb("GA", [128, 16]); LF = sb("LF", [128, 8]); G8 = sb("G8", [128, 8])
        C_b = sb("C_b", [128, 4, 129]); T_f = sb("T_f", [128, 4, 129])
        C_f = sb("C_f", [128, 4, 129]); SINB = sb("SINB", [128, 4, 129])
        FCTX = SINB
        TMPS = T_f
        ESF = sb("ESF", [128, 4]); ESB = sb("ESB", [128, 4]); GT = sb("GT", [128, 8]); WC = sb("WC", [128, 4]); WCB = sb("WCB", [128, 4])
        SNP = [sb(f"SNP{i}", [128, 4, 129], BF16) for i in range(2)]
        RT = [sb(f"RT{i}", [128, 2, 129]) for i in range(2)]
        PQ = [sb(f"PQ{i}", [128, 4, 514]) for i in range(2)]
        uT = sb("uT", [128, 4, 512], BF16)
        HL = sb("HL", [128, 4]); HR = sb("HR", [128, 4])
        WPC = [sb(f"WPC{i}", [128, KC, 512], BF16) for i in range(2)]
        WG = sb("WG", [128, KC, 16], BF16)
        MIXT = al(PQ[0], 0, [128, 8, 512], BF16, nb=8)
        HALO = al(PQ[1], 0, [128, 8, 512], BF16)
        RING = [al(hT, i * 1024, [128, 512], BF16) for i in range(10)]
        HS = al(hT, 10240, [128, 4, 128], F32); HSQ = al(hT, 12288, [128, 4, 128], F32); SG = al(hT, 14336, [128, 512], F32)
        SM = [al(hT, 16384 + i * 1024, [128, 4, 128], BF16) for i in range(2)]
        ND = [al(hT, 18432 + i * 1056, [128, 2, 129], F32) for i in range(2)]
        MO = al(hT, 20544, [128, 4, 128], BF16)
        DT_ = al(uT, 0, [128, 4, 128], BF16); DTT = al(uT, 1024, [128, 4, 128], BF16)
        STR = [al(BIG0, i * 4160, [128, CCW], F32) for i in range(2)]
        CFB = al(BIG0, 8320, [128, 4, 129], BF16); CBC = al(BIG0, 9376, [128, 4, 129], BF16)
        HQ = [al(hT, i * 8192, [128, 8, 512], BF16) for i in range(2)]
        SM8 = [sb(f"SM8_{i}", [128, 8]) for i in range(4)]
        GTQ = sb("GTQ", [128, 8, 8]); GTT = sb("GTT", [128, 8, 8]); ARGS = sb("ARGS", [128, 8, 8]); WQ8 = sb("WQ8", [128, 8, 8])
        TM48 = sb("TM48", [128, 4, 8])

        if ACCT_ONLY:
            tot = sum(acct.values())
            print("SBUF bytes/partition:", tot, "limit", nc.sbuf_top - nc.sbuf_base)
            for k, v in sorted(acct.items(), key=lambda kv: -kv[1])[:40]:
                print("  ", k, v)
            return None
        PS = [T(st.enter_context(nc.psum_tensor(f"ps{i}", [128, 512], F32))) for i in range(7)]
        PSB = T(st.enter_context(nc.psum_tensor("psb", [128, 8, 128], BF16)))
        psi = [0]

        def nps():
            p = PS[psi[0] % 7]
            psi[0] += 1
            return p

        def bl(*ts):
            out = []
            for t in ts:
                if isinstance(t, T):
                    out += t.b
                elif isinstance(t, Buf):
                    out.append(t)
                else:
                    out += list(t)
            return out

        def act(out, in_, func, r, w, bias=None, scale=None):
            kw = {}
            if bias is not None:
                kw["bias"] = bias
            if scale is not None:
                kw["scale"] = scale
            P.op("act", lambda e: e.activation(out, in_, func, **kw), bl(*r), bl(*w))

        def cp(eng, out, in_, r, w):
            if eng == "act":
                P.op("act", lambda e: e.copy(out, in_), bl(*r), bl(*w))
            else:
                P.op(eng, lambda e: e.tensor_copy(out, in_), bl(*r), bl(*w))

        def tt(eng, out, in0, in1, op, r, w):
            P.op(eng, lambda e: e.tensor_tensor(out, in0, in1, op), bl(*r), bl(*w))

        def ts(eng, out, in0, s1, s2, op0, op1, r, w):
            P.op(eng, lambda e: e.tensor_scalar(out, in0, s1, s2, op0, op1), bl(*r), bl(*w))

        def ts1(eng, out, in0, s1, op0, r, w):
            P.op(eng, lambda e: e.tensor_single_scalar(out, in0, s1, op0), bl(*r), bl(*w))

        def stt(eng, out, in0, sc, in1, op0, op1, r, w):
            P.op(eng, lambda e: e.scalar_tensor_tensor(out, in0, sc, in1, op0, op1), bl(*r), bl(*w))

        def mm(out, lhsT, rhs, start, stop, r, w):
            P.op("pe", lambda e: e.matmul(out, lhsT, rhs, start=start, stop=stop), bl(*r), bl(*w))

        def tr(out, in_, ident, r, w):
            P.op("pe", lambda e: e.transpose(out, in_, ident), bl(*r), bl(*w))

        def dma(out, in_, r, w, sem, join=False):
            P.dma("sp", out, in_, bl(*r), bl(*w), sem=sem, join=join)

        def recip(out, in_, r, w):
            P.op("dve", lambda e: e.reciprocal(out, in_), bl(*r), bl(*w))

        def memset(eng, ap, val, w):
            P.op(eng, lambda e: e.memset(ap, val), [], bl(*w))

        memset("pool", identF.t[:], 1.0, [identF])
        P.op("pool", lambda e: e.affine_select(out=identF.t[:], in_=identF.t[:], pattern=[[-1, 128]],
                                               compare_op=ALU.is_equal, fill=0.0, base=0, channel_multiplier=1),
             identF.b, identF.b)
        cp("pool", identB.t[:], identF.t[:], [identF], [identB])
        memset("pool", onesF.t[:], 1.0, [onesF]); memset("pool", onesB.t[:], 1.0, [onesB])
        memset("pool", triF.t[:], 1.0, [triF]); memset("pool", triB.t[:], 1.0, [triB])
        P.op("pool", lambda e: e.affine_select(out=triF.t[:], in_=triF.t[:], pattern=[[1, 128]], compare_op=ALU.is_ge,
                                               fill=0.0, base=0, channel_multiplier=-1), triF.b, triF.b)
        P.op("pool", lambda e: e.affine_select(out=triB.t[:], in_=triB.t[:], pattern=[[-1, 128]], compare_op=ALU.is_ge,
                                               fill=0.0, base=0, channel_multiplier=1), triB.b, triB.b)
        for v in VXT:
            memset("pool", v.t[:, :, 128:129], 1.0, [v])

        dma(flags.t[:], flags_d[:, :], [], [flags], "tb")
        dma(predt.t[:].rearrange("p a b -> p (a b)"), pred_d[:, :], [], [predt], "tb", True)
        dma(mbt.t[:].rearrange("p d a b -> p (d a b)"), mbt_d[:, :], [], [mbt], "tb", True)
        dma(sel.t[:].rearrange("p a b -> p (a b)"), sel_d[:, :], [], [sel], "tb", True)
        dma(invcnt.t[:].rearrange("p a b -> p (a b)"), invcnt_d[:, :], [], [invcnt], "tb", True)
        dma(HNORM.t[:], head_norm.ap().partition_broadcast(128), [], [HNORM], "tb", True)
        dma(GBIAS.t[:], gbias.ap().partition_broadcast(128), [], [GBIAS], "tb", True)
        b0v = BIG0.t[:, 0:NOFF * 128]
        dma(b0v, pm_d[:, :], [], [BIG0], "stg0")
        cp("pool", PM.t[:].rearrange("p a b -> p (a b)"), b0v, [BIG0], [PM])

        vecs = [("c", c_loc, KC), ("cx", c_ctx, KC), ("bm", b_mod, 9 * KC), ("npre", norm_pre, 3 * KC),
                ("npost", norm_post, 3 * KC), ("cw", conv_w, 12), ("cb", conv_b, 4), ("psc", pool_scale, 4)]
        voff = {}
        r0 = 0
        memset("pool", rowsA.t[:], 0.0, [rowsA]); memset("pool", rowsB.t[:], 0.0, [rowsB])
        for name, dten, n in vecs:
            voff[name] = r0
            src = dten.ap().rearrange("(r c) -> r c", c=128)
            a, bnd = r0, r0 + n
            if a < 128:
                e_ = min(bnd, 128)
                dma(rowsA.t[a:e_, :], src[0:e_ - a, :], [], [rowsA], "tb", True)
            if bnd > 128:
                s_ = max(a, 128)
                dma(rowsB.t[s_ - 128:bnd - 128, :], src[s_ - a:n, :], [], [rowsB], "tb", True)
            r0 += n
        assert r0 == NV and NV <= 256
        p_ = nps()
        tr(p_.t[:, 0:128], rowsA.t[:], identF.t[:], [rowsA, identF], [p_])
        na = min(NV, 128)
        cp("dve", VT.t[:, 0:na], p_.t[:, 0:na], [p_], [VT])
        if NV > 128:
            nb_ = NV - 128
            p_ = nps()
            tr(p_.t[:, 0:nb_], rowsB.t[0:nb_, :], identF.t[0:nb_, 0:nb_], [rowsB, identF], [p_])
            cp("dve", VT.t[:, 128:NV], p_.t[:, 0:nb_], [p_], [VT])

        def vcol(name, i):
            c = voff[name] + i
            return VT.t[:, c:c + 1]

        act(SC.t[:, :, 0], VT.t[:, voff["c"]:voff["c"] + KC], AF.Silu, [VT], [SC])
        act(SC.t[:, :, 1], VT.t[:, voff["cx"]:voff["cx"] + KC], AF.Silu, [VT], [SC])

        if KSTOP <= 1:
            P.emit()
            return nc
        stg = [BIG0, BIG1]
        cst = [hT.t[:, 0:2816], hT.t[:, 2816:5632]]
        cstB = [hT.b[0:max(1, JC // 2)], hT.b[max(1, JC // 2):]]
        wi_ = [0]

        def prep(src_aps, width, dst_ap, dstbuf):
            i = wi_[0] % 2
            wi_[0] += 1
            o = 0
            for k, (sap, wd) in enumerate(src_aps):
                dma(stg[i].t[:, o:o + wd] if len(sap.shape) == 2 else stg[i].t[:, o:o + wd].rearrange(
                    "p (a b) -> p a b", a=sap.shape[1]), sap, [], [stg[i]], f"stg{i}", k > 0)
                o += wd
            assert o == width
            cp("pool", cst[i][:, 0:width], stg[i].t[:, 0:width], [stg[i]], cstB[i])
            dma(dst_ap, cst[i][:, 0:width], cstB[i], [dstbuf], f"cso{i}")

        for f in range(2):
            for j in range(JC):
                src = ffn_w_in[f].rearrange("(kc p) c -> p kc c", p=128)
                i = wi_[0] % 2
                wi_[0] += 1
                sv = stg[i].t[:, 0:KC * 256].rearrange("p (kc c) -> p kc c", kc=KC)
                dma(sv[:, :, 0:128], src[:, :, j * 128:(j + 1) * 128], [], [stg[i]], f"stg{i}")
                dma(sv[:, :, 128:256], src[:, :, DFF + j * 128:DFF + (j + 1) * 128], [], [stg[i]], f"stg{i}", True)
                cp("pool", cst[i][:, 0:KC * 256], stg[i].t[:, 0:KC * 256], [stg[i]], cstB[i])
                dma(Win_s[f * JC + j], cst[i][:, 0:KC * 256], cstB[i], [dB["Win"][f * JC + j]], f"cso{i}")
            for m in range(KC):
                src = ffn_w_out[f].rearrange("(jc p) c -> p jc c", p=128)[:, :, m * 128:(m + 1) * 128]
                prep([(src, JC * 128)], JC * 128, Wout_s[f * KC + m], dB["Wout"][f * KC + m])
        wsrc = w_in.ap().rearrange("(kc p) c -> p kc c", p=128)
        wdst = Wi_s.ap().rearrange("p (kc c) -> p kc c", kc=KC)
        c0 = 0
        while c0 < cfg.INC:
            cw_ = min(256, cfg.INC - c0)
            i = wi_[0] % 2
            wi_[0] += 1
            sv = stg[i].t[:, 0:KC * cw_].rearrange("p (kc c) -> p kc c", kc=KC)
            cv = cst[i][:, 0:KC * cw_].rearrange("p (kc c) -> p kc c", kc=KC)
            dma(sv, wsrc[:, :, c0:c0 + cw_], [], [stg[i]], f"stg{i}")
            cp("pool", cst[i][:, 0:KC * cw_], stg[i].t[:, 0:KC * cw_], [stg[i]], cstB[i])
            dma(wdst[:, :, c0:c0 + cw_], cv, cstB[i], [dB["Wi"][0]], f"cso{i}")
            c0 += cw_
        wsrc = w_out.ap().rearrange("(mc p) c -> p mc c", p=128)
        wdst = Wo_s.ap().rearrange("p (mc c) -> p mc c", mc=8)
        for c0 in range(0, D, 256):
            i = wi_[0] % 2
            wi_[0] += 1
            sv = stg[i].t[:, 0:8 * 256].rearrange("p (kc c) -> p kc c", kc=8)
            cv = cst[i][:, 0:8 * 256].rearrange("p (kc c) -> p kc c", kc=8)
            dma(sv, wsrc[:, :, c0:c0 + 256], [], [stg[i]], f"stg{i}")
            cp("pool", cst[i][:, 0:2048], stg[i].t[:, 0:2048], [stg[i]], cstB[i])
            dma(wdst[:, :, c0:c0 + 256], cv, cstB[i], [dB["Wo"][0]], f"cso{i}")
        for dst, srcw in ((wq, w_q), (wk, w_k), (pw, pool_w)):
            i = wi_[0] % 2
            wi_[0] += 1
            sv = stg[i].t[:, 0:512].rearrange("p (h c) -> p h c", h=4)
            dma(sv, srcw.ap().rearrange("h p c -> p h c"), [], [stg[i]], f"stg{i}")
            cp("pool", dst.t[:], sv, [stg[i]], [dst])

        if KSTOP <= 2:
            P.emit()
            return nc
        pm_ = nps()
        pmv = pm_.t[:, 0:18 * KC].rearrange("p (a b) -> p a b", b=2)
        gw = 4 if (9 * KC) % 4 == 0 else 2
        wmsrc = w_mod.ap().rearrange("(kc p) c -> p kc c", p=128)
        for og in range(9 * KC // gw):
            i = wi_[0] % 2
            wi_[0] += 1
            sv = stg[i].t[:, 0:KC * gw * 128].rearrange("p (kc c) -> p kc c", kc=KC)
            dma(sv, wmsrc[:, :, og * gw * 128:(og + 1) * gw * 128], [], [stg[i]], f"stg{i}")
            for o4 in range(gw):
                oc = og * gw + o4
                for kc in range(KC):
                    mm(pmv[:, oc, :], sv[:, kc, o4 * 128:(o4 + 1) * 128], SC.t[:, kc, :], kc == 0, kc == KC - 1,
                       [stg[i], SC], [pm_])
        bm0 = voff["bm"]
        tt("dve", MODT.t[:], pmv, VT.t[:, bm0:bm0 + 9 * KC].unsqueeze(2).to_broadcast([128, 9 * KC, 2]), ALU.add,
           [pm_, VT], [MODT])
        RESW = (0.5, 1.0, 0.5)
        for j in range(3):
            gpre = VT.t[:, voff["npre"] + j * KC: voff["npre"] + (j + 1) * KC].unsqueeze(2).to_broadcast([128, KC, 2])
            gpost = VT.t[:, voff["npost"] + j * KC: voff["npost"] + (j + 1) * KC].unsqueeze(2).to_broadcast([128, KC, 2])
            ts1("dve", AIN.t[:, j], MODT.t[:, (3 * j + 1) * KC:(3 * j + 2) * KC, :], 1.0, ALU.add, [MODT], [AIN])
            tt("dve", AIN.t[:, j], AIN.t[:, j], gpre, ALU.mult, [AIN, VT], [AIN])
            tt("dve", AOUT.t[:, j], MODT.t[:, (3 * j + 2) * KC:(3 * j + 3) * KC, :], gpost, ALU.mult, [MODT, VT], [AOUT])
            ts1("dve", AOUT.t[:, j], AOUT.t[:, j], RESW[j], ALU.mult, [AOUT], [AOUT])

        def ain(j, kc, mi):
            return AIN.t[:, j, kc, mi:mi + 1]

        def bin_(j, kc, mi):
            return MODT.t[:, 3 * j * KC + kc, mi:mi + 1]

        def aout(j, kc, mi):
            return AOUT.t[:, j, kc, mi:mi + 1]

        if KSTOP <= 3:
            P.emit()
            return nc
        xT = BIG1.t[:, 0:KC * 512].rearrange("p (kc n) -> p kc n", kc=KC)
        zT = BIG0.t[:, 0:KC * 512].rearrange("p (kc n) -> p kc n", kc=KC)
        xTb, zTb = BIG1.b, BIG0.b
        hTv = hT.t[:, 0:JC * 512].rearrange("p (j n) -> p j n", j=JC)

        def load_xT(src_rows, N):
            for bi in range(N // 128):
                xi = XIO[bi % 2]
                dma(xi.t[:], src_rows[bi * 128:(bi + 1) * 128, :], [], [xi], f"xin{bi % 2}")
                for k0 in range(0, KC, 4):
                    kn = min(4, KC - k0)
                    p = nps()
                    for k in range(kn):
                        tr(p.t[:, k * 128:(k + 1) * 128], xi.t[:, (k0 + k) * 128:(k0 + k + 1) * 128], identF.t[:],
                           [xi, identF], [p])
                    cp("dve", xT[:, k0:k0 + kn, bi * 128:(bi + 1) * 128],
                       p.t[:, 0:kn * 128].rearrange("p (k n) -> p k n", k=kn), [p], xTb[k0:k0 + kn])

        def stats(src, srcb, N):
            p = nps()
            for kc in range(KC):
                s = sq[kc % 2]
                act(s.t[:, 0:N], src[:, kc, 0:N], AF.Square, [srcb[kc]], [s])
                mm(p.t[:, 0:N], onesB.t[:], s.t[:, 0:N], kc == 0, kc == KC - 1, [onesB, s], [p])
            ts("dve", rstd.t[:, 0:N], p.t[:, 0:N], 1.0 / D, EPS, ALU.mult, ALU.add, [p], [rstd])
            act(rstd.t[:, 0:N], rstd.t[:, 0:N], AF.Sqrt, [rstd], [rstd])
            recip(rstd.t[:, 0:N], rstd.t[:, 0:N], [rstd], [rstd])

        def ada_in(src, srcb, j, segs):
            for kc in range(KC):
                for (lo, hi, mi) in segs:
                    tm = tmpf[kc % 2]
                    tt("dve", tm.t[:, lo:hi], src[:, kc, lo:hi], rstd.t[:, lo:hi], ALU.mult, [srcb[kc], rstd], [tm])
                    act(yT.t[:, kc, lo:hi], tm.t[:, lo:hi], AF.Identity, [tm, AIN, MODT], [yT.b[kc]],
                        bias=bin_(j, kc, mi), scale=ain(j, kc, mi))

        def ada_out(j, segs):
            for kc in range(KC):
                for (lo, hi, mi) in segs:
                    tm = tmpf[kc % 2]
                    tt("dve", tm.t[:, lo:hi], zT[:, kc, lo:hi], rstd.t[:, lo:hi], ALU.mult, [zTb[kc], rstd], [tm])
                    stt("dve", xT[:, kc, lo:hi], tm.t[:, lo:hi], aout(j, kc, mi), xT[:, kc, lo:hi], ALU.mult, ALU.add,
                        [tm, AOUT, xTb[kc]], [xTb[kc]])

        wctr = [0, 0]

        def ffn(f, N):
            for j in range(JC):
                wb = WIN[wctr[0] % 3]
                wctr[0] += 1
                dma(wb.t[:].rearrange("p k c -> p (k c)"), Win_s[f * JC + j], [dB["Win"][f * JC + j]], [wb],
                    f"win{(wctr[0] - 1) % 3}")
                pa, pb = nps(), nps()
                for kc in range(KC):
                    mm(pa.t[:, 0:N], wb.t[:, kc, 0:128], yT.t[:, kc, 0:N], kc == 0, kc == KC - 1, [wb, yT.b[kc]], [pa])
                for kc in range(KC):
                    mm(pb.t[:, 0:N], wb.t[:, kc, 128:256], yT.t[:, kc, 0:N], kc == 0, kc == KC - 1, [wb, yT.b[kc]], [pb])
                sa = tmpf[2]
                act(sa.t[:, 0:N], pa.t[:, 0:N], AF.Silu, [pa], [sa])
                tt("dve", hTv[:, j, 0:N], sa.t[:, 0:N], pb.t[:, 0:N], ALU.mult, [sa, pb], [hT.b[j]])
            for m in range(KC):
                wo = WOUT[wctr[1] % 2]
                wctr[1] += 1
                dma(wo.t[:].rearrange("p j c -> p (j c)"), Wout_s[f * KC + m], [dB["Wout"][f * KC + m]], [wo],
                    f"wout{(wctr[1] - 1) % 2}")
                pz = nps()
                for j in range(JC):
                    mm(pz.t[:, 0:N], wo.t[:, j, :], hTv[:, j, 0:N], j == 0, j == JC - 1, [wo, hT.b[j]], [pz])
                cp("act", zT[:, m, 0:N], pz.t[:, 0:N], [pz], [zTb[m]])

        QK0, V0, O0, G0, P0 = 0, 512, 1024, 1536, 1552
        wiv = Wi_s.ap().rearrange("p (kc c) -> p kc c", kc=KC)

        def inproj_fm(pq, N, off):
            for h in range(4):
                wb = WIN[wctr[0] % 3]
                wctr[0] += 1
                dma(wb.t[:, :, 0:128], wiv[:, :, QK0 + h * 128:QK0 + (h + 1) * 128], [dB["Wi"][0]], [wb],
                    f"win{(wctr[0] - 1) % 3}")
                p = nps()
                for kc in range(KC):
                    mm(p.t[:, 0:N], wb.t[:, kc, 0:128], yT.t[:, kc, 0:N], kc == 0, kc == KC - 1, [wb, yT.b[kc]], [p])
                cp("act", pq.t[:, h, off:off + N], p.t[:, 0:N], [p], [pq])

        gp_cnt = [0]

        def gate_pack(ci, lhs_chunk_ap, lhs_bufs):
            p = nps()
            for kc in range(KC):
                mm(p.t[:, 0:16], lhs_chunk_ap(kc), WG.t[:, kc, :], kc == 0, kc == KC - 1, [WG] + lhs_bufs, [p])
            tt("dve", GA.t[:], p.t[:, 0:16], GBIAS.t[:], ALU.add, [p, GBIAS], [GA])
            act(LF.t[:], GA.t[:, 8:16], AF.Exp, [GA], [LF], scale=-1.0)
            act(LF.t[:], LF.t[:], AF.Ln, [LF], [LF], bias=1.0)
            ts1("dve", LF.t[:], LF.t[:], -1.0, ALU.mult, [LF], [LF])
            p2 = nps()
            mm(p2.t[:, 0:4], triF.t[:], LF.t[:, 0:4], True, True, [triF, LF], [p2])
            mm(p2.t[:, 4:8], triB.t[:], LF.t[:, 4:8], True, True, [triB, LF], [p2])
            mm(p2.t[:, 8:16], onesF.t[:], LF.t[:], True, True, [onesF, LF], [p2])
            gb = [GP.b[ci]]
            tt("dve", G8.t[:], GA.t[:, 0:8], p2.t[:, 0:8], ALU.subtract, [GA, p2], [G8])
            act(GP.t[:, ci, 0:8], G8.t[:], AF.Exp, [G8], gb)
            act(GP.t[:, ci, 8:16], p2.t[:, 0:8], AF.Exp, [p2], gb)
            act(GP.t[:, ci, 16:24], p2.t[:, 8:16], AF.Exp, [p2], gb)
            cp("dve", GP.t[:, ci, 24:32], p2.t[:, 8:16], [p2], gb)

        def inproj_tm(N, nchunks, chunk0, vx_list, full, gblk0):
            dma(WG.t[:], wiv[:, :, G0:G0 + 16], [dB["Wi"][0]], [WG], "wg")
            for ci in range(nchunks):
                gate_pack(chunk0 + ci, lambda kc, ci=ci: yT.t[:, kc, ci * 128:(ci + 1) * 128], list(yT.b))
            pieces = [("v", V0)] + ([("o", O0), ("p", P0)] if full else [])
            if 'op' in KOFF:
                pieces = pieces[0:1]
            if 'p' in KOFF:
                pieces = pieces[0:2]
            for nm, c0 in pieces:
                wb = WPC[wctr[0] % 2]
                wctr[0] += 1
                dma(wb.t[:], wiv[:, :, c0:c0 + 512], [dB["Wi"][0]], [wb], f"wpc{(wctr[0] - 1) % 2}")
                for ci in range(nchunks):
                    p = nps()
                    for kc in range(KC):
                        mm(p.t[:], yT.t[:, kc, ci * 128:(ci + 1) * 128], wb.t[:, kc, :], kc == 0, kc == KC - 1,
                           [wb, yT.b[kc]], [p])
                    gb = gblk0 + ci
                    if nm == "v":
                        vx = vx_list[ci]
                        cp("act", vx.t[:, :, 0:128], p.t[:].rearrange("p (h c) -> p h c", h=4), [p], [vx])
                        if full and 'v' not in KOFF:
                            dma(V_s[gb], vx.t[:].rearrange("p h c -> p (h c)"), [vx], [dB["V"][gb]], f"vs{ci % 2}")
                    elif nm == "o":
                        o = OT[ci % 2]
                        cp("act", o.t[:], p.t[:], [p], [o])
                        if 'o' not in KOFF:
                            dma(O_s[gb], o.t[:], [o], [dB["O"][gb]], f"os{ci % 2}")
                    else:
                        pt, ptb = PT[ci % 2], PTb[ci % 2]
                        if 'pa' not in KOFF:
                            cp("act", pt.t[:], p.t[:], [p], [pt])
                        if 'pb' not in KOFF:
                            cp("act", ptb.t[:], p.t[:], [p], [ptb])
                        if 'pl' not in KOFF:
                            dma(PL_s[gb], pt.t[:], [pt], [dB["PL"][gb]], f"pls{ci % 2}")
                            dma(PLb_s[gb], ptb.t[:], [ptb], [dB["PLb"][gb]], f"plb{ci % 2}")
                        if gb < 4 and 'cc2' not in KOFF:
                            dma(cc2_in[:, gb * 512:(gb + 1) * 512], ptb.t[:], [ptb], [dB["cc2i"][0]], f"plc{ci % 2}")
                        if gb >= NB - 4 and 'cc2' not in KOFF:
                            k = 4 + gb - (NB - 4)
                            dma(cc2_in[:, k * 512:(k + 1) * 512], ptb.t[:], [ptb], [dB["cc2i"][0]], f"pld{ci % 2}")

        def conv_u(pq, N):
            for h in range(4):
                tm = tmpf[h % 2]
                ts("dve", tm.t[:, 0:N], pq.t[:, h, 0:N], vcol("cw", 0 * 4 + h), vcol("cb", h), ALU.mult, ALU.add,
                   [pq, VT], [tm])
                stt("dve", tm.t[:, 0:N], pq.t[:, h, 1:N + 1], vcol("cw", 1 * 4 + h), tm.t[:, 0:N], ALU.mult, ALU.add,
                    [pq, VT, tm], [tm])
                stt("dve", tm.t[:, 0:N], pq.t[:, h, 2:N + 2], vcol("cw", 2 * 4 + h), tm.t[:, 0:N], ALU.mult, ALU.add,
                    [pq, VT, tm], [tm])
                act(uT.t[:, h, 0:N], tm.t[:, 0:N], AF.Silu, [tm], [uT])

        KSC = 128.0 ** -0.5

        def ktok_chunk(ci_local, kt):
            p = nps()
            for h in range(4):
                mm(p.t[:, h * 128:(h + 1) * 128], uT.t[:, h, ci_local * 128:(ci_local + 1) * 128], wk.t[:, h, :], True, True,
                   [uT, wk], [p])
            act(kt.t[:].rearrange("p h c -> p (h c)"), p.t[:], AF.Copy, [p], [kt], scale=KSC)

        def ckv(kt, vw, pair):
            p = nps()
            pv = p.t[:, 0:258].rearrange("p (h c) -> p h c", h=2)
            for hh in range(2):
                h = pair * 2 + hh
                mm(pv[:, hh, :], kt.t[:, h, :], vw.t[:, h, :], True, True, [kt, vw], [p])
            return p, pv

        def vw_make(vw, vx, ci, d):
            tt("dve", vw.t[:], vx.t[:], GP.t[:, ci, d * 4:(d + 1) * 4].unsqueeze(2).to_broadcast([128, 4, 129]), ALU.mult,
               [vx, GP.b[ci]], [vw])

        def state_step_A(ci, kt, vx, snap_gb):
            if snap_gb is not None and 'snap' not in KOFF:
                sn = SNP[snap_gb % 2]
                cp("act", sn.t[:], C_b.t[:], [C_b], [sn])
                dma(SNAP_s[snap_gb], sn.t[:].rearrange("p h c -> p (h c)"), [sn], [dB["SNAP"][snap_gb]], f"sn{snap_gb % 2}")
                cp("pool", GPX.t[:, ci, :], ESB.t[:], [ESB], [GPX.b[ci]])
                tt("pool", WCB.t[:], ESB.t[:], GP.t[:, ci, 20:24], ALU.mult, [ESB, GP.b[ci]], [WCB])
                cp("pool", ESB.t[:], WCB.t[:], [WCB], [ESB])
            vw = VW[0]
            vw_make(vw, vx, ci, 1)
            if KSTOP <= 3.86:
                return
            for pair in range(2):
                p, pv = ckv(kt, vw, pair)
                if KSTOP <= 3.87:
                    return
                rt = RT[pair]
                tt("dve", rt.t[:], pv, C_b.t[:, pair * 2:pair * 2 + 2, :], ALU.add, [p, C_b], [rt])
                tt("dve", C_b.t[:, pair * 2:pair * 2 + 2, :], rt.t[:],
                   GP.t[:, ci, 20 + pair * 2:22 + pair * 2].unsqueeze(2).to_broadcast([128, 2, 129]), ALU.mult,
                   [rt, GP.b[ci]], [C_b])
            if KSTOP <= 3.88:
                return
            vw = VW[1]
            vw_make(vw, vx, ci, 0)
            tt("pool", WC.t[:], ESF.t[:], GP.t[:, ci, 16:20], ALU.mult, [ESF, GP.b[ci]], [WC])
            cp("pool", ESF.t[:], WC.t[:], [WC], [ESF])
            if KSTOP <= 3.89:
                return
            for pair in range(2):
                p, pv = ckv(kt, vw, pair)
                rt = RT[pair]
                tt("dve", rt.t[:], pv, WC.t[:, pair * 2:pair * 2 + 2].unsqueeze(2).to_broadcast([128, 2, 129]), ALU.mult,
                   [p, WC], [rt])
                tt("dve", T_f.t[:, pair * 2:pair * 2 + 2, :], T_f.t[:, pair * 2:pair * 2 + 2, :], rt.t[:], ALU.add,
                   [T_f, rt], [T_f])

        SEG_MAIN = [(0, 512, 0)]
        SEG_AUX = [(0, cfg.CTX, 1), (cfg.CTX, NA, 0)]

        for t_ in (C_b, T_f, GT):
            memset("pool", t_.t[:], 0.0, [t_])
        memset("pool", ESF.t[:], 1.0, [ESF]); memset("pool", ESB.t[:], 1.0, [ESB])
        load_xT(x_aux.ap(), NA)
        if KSTOP <= 3.1:
            P.emit()
            return nc
        stats(xT, xTb, NA)
        if KSTOP <= 3.2:
            P.emit()
            return nc
        ada_in(xT, xTb, 0, SEG_AUX)
        if KSTOP <= 3.3:
            P.emit()
            return nc
        ffn(0, NA)
        if KSTOP <= 3.4:
            P.emit()
            return nc
        stats(zT, zTb, NA)
        ada_out(0, SEG_AUX)
        stats(xT, xTb, NA)
        ada_in(xT, xTb, 1, SEG_AUX)
        if KSTOP <= 3.5:
            P.emit()
            return nc
        pqa = PQ[1]
        memset("pool", pqa.t[:], 0.0, [pqa])
        inproj_fm(pqa, NA, 1)
        if KSTOP <= 3.6:
            P.emit()
            return nc
        ts1("dve", HL.t[:], pqa.t[:, :, 1 + cfg.CTX], flags.t[:, 2:3], ALU.mult, [pqa, flags], [HL])
        ts1("dve", HR.t[:], pqa.t[:, :, 2 + cfg.CTX], flags.t[:, 3:4], ALU.mult, [pqa, flags], [HR])
        memset("dve", pqa.t[:, :, 1 + cfg.CTX:2 + cfg.CTX], 0.0, [pqa])
        inproj_tm(NA, CC, NB, VXT[0:CC], False, 0)
        if KSTOP <= 3.7:
            P.emit()
            return nc
        conv_u(pqa, cfg.CTX)
        if KSTOP <= 3.8:
            P.emit()
            return nc
        for ci in reversed(range(CC)):
            kt = KTOK[ci % 2]
            ktok_chunk(ci, kt)
            if KSTOP <= 3.85:
                P.emit()
                return nc
            state_step_A(NB + ci, kt, VXT[ci], None)
            if KSTOP <= 3.9:
                P.emit()
                return nc
        cp("pool", FCTX.t[:], T_f.t[:], [T_f], [FCTX])
        memset("pool", T_f.t[:], 0.0, [T_f])
        ts1("dve", C_b.t[:], C_b.t[:], flags.t[:, 1:2], ALU.mult, [C_b, flags], [C_b])
        memset("pool", ESF.t[:], 1.0, [ESF]); memset("pool", ESB.t[:], 1.0, [ESB])

        if KSTOP <= 4:
            P.emit()
            return nc
        def prep_tile(i):
            pq = PQ[i % 2]
            conv_u(pq, 512)
            for h in range(4):
                p = nps()
                mm(p.t[:], wq.t[:, h, :], uT.t[:, h, :], True, True, [wq, uT], [p])
                cp("act", QT.t[:, h, :], p.t[:], [p], [QT])
                p = nps()
                mm(p.t[:], wk.t[:, h, :], uT.t[:, h, :], True, True, [wk, uT], [p])
                act(KT.t[:, h, :], p.t[:], AF.Copy, [p], [KT], scale=KSC)
            dma(QT_s[i], QT.t[:].rearrange("p h n -> p (h n)"), [QT], [dB["QT"][i]], "qts")
            dma(KT_s[i], KT.t[:].rearrange("p h n -> p (h n)"), [KT], [dB["KT"][i]], "kts")
            if KSTOP <= 4.4:
                return
            for cl in reversed(range(4)):
                gb = i * 4 + cl
                kt = KTOK[cl % 2]
                ktok_chunk(cl, kt)
                dma(KTOK_s[gb], kt.t[:].rearrange("p h c -> p (h c)"), [kt], [dB["KTOK"][gb]], f"kk{cl % 2}")
                state_step_A(gb, kt, VXT[(i % 2) * 4 + cl], gb)
                if 'gt' not in KOFF:
                    tt("pool", GT.t[:], GT.t[:], GP.t[:, gb, 24:32], ALU.add, [GT, GP.b[gb]], [GT])

        for step in range(NT + 1):
            i = NT - 1 - step
            if i >= 0:
                load_xT(x_loc[i * 512:(i + 1) * 512, :], 512)
                stats(xT, xTb, 512)
                ada_in(xT, xTb, 0, SEG_MAIN)
                ffn(0, 512)
                stats(zT, zTb, 512)
                ada_out(0, SEG_MAIN)
                dma(X1_s[i], BIG1.t[:, 0:KC * 512], xTb, [dB["X1"][i]], "x1s")
                if KSTOP <= 4.1:
                    P.emit()
                    return nc
                stats(xT, xTb, 512)
                ada_in(xT, xTb, 1, SEG_MAIN)
                pq = PQ[i % 2]
                inproj_fm(pq, 512, 1)
                if i == NT - 1:
                    cp("pool", pq.t[:, :, 513], HR.t[:], [HR], [pq])
                else:
                    pqn = PQ[(i + 1) % 2]
                    cp("pool", pq.t[:, :, 513], pqn.t[:, :, 1], [pqn], [pq])
                    cp("pool", pqn.t[:, :, 0], pq.t[:, :, 512], [pq], [pqn])
                if i == 0:
                    cp("pool", pq.t[:, :, 0], HL.t[:], [HL], [pq])
                if KSTOP <= 4.2:
                    P.emit()
                    return nc
                inproj_tm(512, 4, i * 4, VXT[(i % 2) * 4:(i % 2) * 4 + 4], True, i * 4)
                if KSTOP <= 4.3:
                    P.emit()
                    return nc
            if i + 1 <= NT - 1:
                prep_tile(i + 1)
        if KSTOP <= 5:
            P.emit()
            return nc
        tt("dve", C_f.t[:], FCTX.t[:], ESF.t[:].unsqueeze(2).to_broadcast([128, 4, 129]), ALU.mult, [FCTX, ESF], [C_f])
        stt("dve", T_f.t[:], C_f.t[:], flags.t[:, 0:1], T_f.t[:], ALU.mult, ALU.add, [C_f, flags, T_f], [T_f])
        dma(cc1_in[:, 0:516], T_f.t[:].rearrange("p h c -> p (h c)"), [T_f], [dB["cc1i"][0]], "cc1w")
        dma(cc1_in[:, 516:1032], C_b.t[:].rearrange("p h c -> p (h c)"), [C_b], [dB["cc1i"][0]], "cc1w", True)
        dma(cc1_in[:, 1032:1040], GT.t[:], [GT], [dB["cc1i"][0]], "cc1w", True)
        P.cc(lambda e: e.collective_compute("AllGather", ALU.bypass, replica_groups=[list(range(NCORES))],
                                            ins=[cc1_in.ap().opt()], outs=[cc1_out.ap().opt()]),
             bl(dB["cc1i"]), bl(dB["cc1o"]), sem="cc1")
        P.cc(lambda e: e.collective_compute("AllGather", ALU.bypass, replica_groups=[list(range(NCORES))],
                                            ins=[cc2_in.ap().opt()], outs=[cc2_out.ap().opt()]),
             bl(dB["cc2i"]), bl(dB["cc2o"]), sem="cc2")

        if KSTOP <= 6:
            P.emit()
            return nc
        g1 = cc1_out.ap().rearrange("(q p) c -> p q c", p=128)
        dma(GTQ.t[:], g1[:, :, 1032:1040], [dB["cc1o"][0]], [GTQ], "gtq")
        cp("dve", GTT.t[:], GTQ.t[:].rearrange("p q k -> p k q"), [GTQ], [GTT])
        for d in range(2):
            for q in range(8):
                tt("dve", TM48.t[:], GTT.t[:, d * 4:(d + 1) * 4, :], mbt.t[:, d, q, :].unsqueeze(1).to_broadcast([128, 4, 8]),
                   ALU.mult, [GTT, mbt], [TM48])
                P.op("dve", lambda e, q=q, d=d: e.tensor_reduce(ARGS.t[:, q, d * 4:(d + 1) * 4], TM48.t[:], AX.X, ALU.add),
                     bl(TM48), bl(ARGS))
        act(WQ8.t[:], ARGS.t[:], AF.Exp, [ARGS], [WQ8])
        tt("dve", WQ8.t[:], WQ8.t[:], predt.t[:], ALU.mult, [WQ8, predt], [WQ8])
        ts1("dve", C_f.t[:], FCTX.t[:], flags.t[:, 0:1], ALU.mult, [FCTX, flags], [C_f])
        memset("pool", SINB.t[:], 0.0, [SINB])
        for q in range(8):
            s_ = STR[q % 2]
            dma(s_.t[:], cc1_out[q * 128:(q + 1) * 128, :], [dB["cc1o"][0]], [s_], f"str{q % 2}")
            for d, dst in ((0, C_f), (1, SINB)):
                sv = s_.t[:, d * 516:(d + 1) * 516].rearrange("p (h c) -> p h c", h=4)
                tt("dve", TMPS.t[:], sv, WQ8.t[:, q, d * 4:(d + 1) * 4].unsqueeze(2).to_broadcast([128, 4, 129]), ALU.mult,
                   [s_, WQ8], [TMPS])
                tt("dve", dst.t[:], dst.t[:], TMPS.t[:], ALU.add, [dst, TMPS], [dst])
        memset("pool", HALO.t[:], 0.0, [HALO])
        for q in range(8):
            hq = HQ[q % 2]
            dma(hq.t[:].rearrange("p a b -> p (a b)"), cc2_out[q * 128:(q + 1) * 128, :], [dB["cc2o"][0]], [hq], f"hq{q % 2}")
            stt("dve", HALO.t[:, 0:4, :], hq.t[:, 4:8, :], sel.t[:, 0, q:q + 1], HALO.t[:, 0:4, :], ALU.mult, ALU.add,
                [hq, sel, HALO], [HALO])
            stt("dve", HALO.t[:, 4:8, :], hq.t[:, 0:4, :], sel.t[:, 1, q:q + 1], HALO.t[:, 4:8, :], ALU.mult, ALU.add,
                [hq, sel, HALO], [HALO])

        if KSTOP <= 7:
            P.emit()
            return nc
        og = {g: [(idx, dl) for idx, (gg, dl) in enumerate(offs) if gg == g] for g in range(4)}
        ring_loaded = [-1]
        hsv = HS.t[:]
        hnv = HNORM.t[:].rearrange("p (h c) -> p h c", h=4)

        def mixer(gb, cl):
            ci = gb
            kt, vx, ot, sn = KTOK[cl % 2], VXT[cl % 2], OT[cl % 2], SNP[cl % 2]
            cs = slice(cl * 128, (cl + 1) * 128)
            p = nps()
            pk = p.t[:].rearrange("p (h c) -> p h c", h=4)
            for h in range(4):
                mm(pk[:, h, :], KT.t[:, h, cs], QT.t[:, h, cs], True, True, [KT, QT], [p])
            tt("dve", SM[0].t[:], pk, triF.t[:].unsqueeze(1).to_broadcast([128, 4, 128]), ALU.mult, [p, triF], [SM[0]])
            tt("dve", SM[1].t[:], pk, triB.t[:].unsqueeze(1).to_broadcast([128, 4, 128]), ALU.mult, [p, triB], [SM[1]])
            cp("act", CFB.t[:], C_f.t[:], [C_f], [CFB])
            tt("pool", CBC.t[:], SINB.t[:], GPX.t[:, ci, :].unsqueeze(2).to_broadcast([128, 4, 129]), ALU.mult,
               [SINB, GPX.b[ci]], [CBC])
            for d in range(2):
                vw_make(VW[d], vx, ci, d)
            for d in range(2):
                vw = VW[d]
                for pair in range(2):
                    p = nps()
                    pv = p.t[:, 0:258].rearrange("p (h c) -> p h c", h=2)
                    for hh in range(2):
                        h = pair * 2 + hh
                        mm(pv[:, hh, :], SM[d].t[:, h, :], vw.t[:, h, :], True, False, [SM[d], vw], [p])
                        if d == 0:
                            mm(pv[:, hh, :], QT.t[:, h, cs], CFB.t[:, h, :], False, True, [QT, CFB], [p])
                        else:
                            mm(pv[:, hh, :], QT.t[:, h, cs], sn.t[:, h, :], False, False, [QT, sn], [p])
                            mm(pv[:, hh, :], QT.t[:, h, cs], CBC.t[:, h, :], False, True, [QT, CBC], [p])
                    nd = ND[pair]
                    e0 = 8 + d * 4 + pair * 2
                    tt("dve", nd.t[:], pv, GP.t[:, ci, e0:e0 + 2].unsqueeze(2).to_broadcast([128, 2, 129]), ALU.mult,
                       [p, GP.b[ci]], [nd])
                    a8, r8 = SM8[0], SM8[1]
                    act(a8.t[:, 0:2], nd.t[:, :, 128], AF.Abs, [nd], [a8])
                    ts1("dve", a8.t[:, 0:2], a8.t[:, 0:2], 1.0, ALU.max, [a8], [a8])
                    recip(r8.t[:, 0:2], a8.t[:, 0:2], [a8], [r8])
                    for hh in range(2):
                        h = pair * 2 + hh
                        if d == 0:
                            ts1("dve", HS.t[:, h, :], nd.t[:, hh, 0:128], r8.t[:, hh:hh + 1], ALU.mult, [nd, r8], [HS])
                        else:
                            stt("dve", HS.t[:, h, :], nd.t[:, hh, 0:128], r8.t[:, hh:hh + 1], HS.t[:, h, :], ALU.mult, ALU.add,
                                [nd, r8, HS], [HS])
            for pair in range(2):
                p, pv = ckv(kt, VW[0], pair)
                rt = RT[pair]
                tt("dve", rt.t[:], pv, C_f.t[:, pair * 2:pair * 2 + 2, :], ALU.add, [p, C_f], [rt])
                tt("dve", C_f.t[:, pair * 2:pair * 2 + 2, :], rt.t[:],
                   GP.t[:, ci, 16 + pair * 2:18 + pair * 2].unsqueeze(2).to_broadcast([128, 2, 129]), ALU.mult,
                   [rt, GP.b[ci]], [C_f])
            tt("pool", HSQ.t[:], HS.t[:], HS.t[:], ALU.mult, [HS], [HSQ])
            s2, s3 = SM8[2], SM8[3]
            P.op("dve", lambda e: e.tensor_reduce(s2.t[:, 0:4], HSQ.t[:], AX.X, ALU.add), bl(HSQ), bl(s2))
            ts("dve", s2.t[:, 0:4], s2.t[:, 0:4], 1.0 / 128, EPS, ALU.mult, ALU.add, [s2], [s2])
            act(s2.t[:, 0:4], s2.t[:, 0:4], AF.Sqrt, [s2], [s2])
            recip(s3.t[:, 0:4], s2.t[:, 0:4], [s2], [s3])
            tt("dve", HS.t[:], HS.t[:], s3.t[:, 0:4].unsqueeze(2).to_broadcast([128, 4, 128]), ALU.mult, [HS, s3], [HS])
            tt("dve", HS.t[:], HS.t[:], hnv, ALU.mult, [HS, HNORM], [HS])
            act(SG.t[:], ot.t[:], AF.Sigmoid, [ot], [SG])
            tt("dve", MO.t[:], HS.t[:], SG.t[:].rearrange("p (h c) -> p h c", h=4), ALU.mult, [HS, SG], [MO])
            for h in range(4):
                tr(PSB.t[:, h, :], MO.t[:, h, :], identB.t[:], [MO, identB], [PSB])
            cp("act", MIXT.t[:, 0:4, cs], PSB.t[:, 0:4, :], [PSB], MIXT.b[0:4])

        def pool_chunk(gb, cl):
            cs = slice(cl * 128, (cl + 1) * 128)
            for blk in range(ring_loaded[0] + 1, min(gb + 4, NB - 1) + 1):
                rg = RING[blk % 10]
                dma(rg.t[:], PLb_s[blk], [dB["PLb"][blk]], [rg], f"rg{blk % 10}")
                ring_loaded[0] = blk
            p = nps()
            pv4 = p.t[:].rearrange("p (g c) -> p g c", g=4)
            for g in range(4):
                lst = og[g]
                for n, (idx, dl) in enumerate(lst):
                    blk = gb + dl
                    if blk < 0:
                        src, sbuf = HALO.t[:, 4 + blk, g * 128:(g + 1) * 128], HALO
                    elif blk >= NB:
                        src, sbuf = HALO.t[:, 4 + blk - NB, g * 128:(g + 1) * 128], HALO
                    else:
                        src, sbuf = RING[blk % 10].t[:, g * 128:(g + 1) * 128], RING[blk % 10]
                    mm(pv4[:, g, :], PM.t[:, idx, :], src, n == 0, n == len(lst) - 1, [PM, sbuf], [p])
            tm = tmpf[0]
            tmv = tm.t[:].rearrange("p (g c) -> p g c", g=4)
            tt("dve", tmv, pv4, invcnt.t[:, gb, :].unsqueeze(2).to_broadcast([128, 4, 128]), ALU.mult, [p, invcnt], [tm])
            pt = PT[cl % 2]
            tt("dve", DT_.t[:], tmv, pt.t[:].rearrange("p (g c) -> p g c", g=4), ALU.subtract, [tm, pt], [DT_])
            for g in range(4):
                tr(PSB.t[:, 4 + g, :], DT_.t[:, g, :], identB.t[:], [DT_, identB], [PSB])
            cp("act", DTT.t[:], PSB.t[:, 4:8, :], [PSB], [DTT])
            for g in range(4):
                p = nps()
                mm(p.t[:, 0:128], pw.t[:, g, :], DTT.t[:, g, :], True, True, [pw, DTT], [p])
                act(MIXT.t[:, 4 + g, cs], p.t[:, 0:128], AF.Copy, [p, VT], [MIXT.b[4 + g]], scale=vcol("psc", g))

        wov_src = Wo_s.ap().rearrange("p (mc c) -> p mc c", mc=8)
        for i in range(NT):
            ring_loaded[0] = max(i * 4 - 5, -1)
            dma(BIG1.t[:, 0:KC * 512], X1_s[i], [dB["X1"][i]], xTb, "x1l")
            dma(QT.t[:].rearrange("p h n -> p (h n)"), QT_s[i], [dB["QT"][i]], [QT], "qtl")
            dma(KT.t[:].rearrange("p h n -> p (h n)"), KT_s[i], [dB["KT"][i]], [KT], "ktl")
            for cl in range(4):
                gb = i * 4 + cl
                dma(KTOK[cl % 2].t[:].rearrange("p h c -> p (h c)"), KTOK_s[gb], [dB["KTOK"][gb]], [KTOK[cl % 2]], f"kkl{cl % 2}")
                dma(VXT[cl % 2].t[:].rearrange("p h c -> p (h c)"), V_s[gb], [dB["V"][gb]], [VXT[cl % 2]], f"vl{cl % 2}")
                dma(OT[cl % 2].t[:], O_s[gb], [dB["O"][gb]], [OT[cl % 2]], f"ol{cl % 2}")
                dma(SNP[cl % 2].t[:].rearrange("p h c -> p (h c)"), SNAP_s[gb], [dB["SNAP"][gb]], [SNP[cl % 2]], f"snl{cl % 2}")
                dma(PT[cl % 2].t[:], PL_s[gb], [dB["PL"][gb]], [PT[cl % 2]], f"pll{cl % 2}")
                mixer(gb, cl)
                pool_chunk(gb, cl)
            for m in range(KC):
                wb = WPC[wctr[0] % 2]
                wctr[0] += 1
                wbv = wb.t[:].rearrange("p k c -> p (k c)")[:, 0:1024].rearrange("p (mc c) -> p mc c", mc=8)
                dma(wbv, wov_src[:, :, m * 128:(m + 1) * 128], [dB["Wo"][0]], [wb], f"wpc{(wctr[0] - 1) % 2}")
                pz = nps()
                for mc in range(8):
                    mm(pz.t[:], wbv[:, mc, :], MIXT.t[:, mc, :], mc == 0, mc == 7, [wb, MIXT.b[mc]], [pz])
                cp("act", zT[:, m, :], pz.t[:], [pz], [zTb[m]])
            stats(zT, zTb, 512)
            ada_out(1, SEG_MAIN)
            stats(xT, xTb, 512)
            ada_in(xT, xTb, 2, SEG_MAIN)
            ffn(1, 512)
            stats(zT, zTb, 512)
            ada_out(2, SEG_MAIN)
            for bi in range(4):
                xo = XIO[bi % 2]
                for k0 in range(0, KC, 4):
                    kn = min(4, KC - k0)
                    p = nps()
                    for k in range(kn):
                        tr(p.t[:, k * 128:(k + 1) * 128], xT[:, k0 + k, bi * 128:(bi + 1) * 128], identF.t[:],
                           [xTb[k0 + k], identF], [p])
                    cp("act", xo.t[:, k0 * 128:(k0 + kn) * 128], p.t[:, 0:kn * 128], [p], [xo])
                r0_ = i * 512 + bi * 128
                dma(out_d[r0_:r0_ + 128, :], xo.t[:], [xo], [dB["out"][0]], f"xin{bi % 2}")
        P.op("sp", None, bl(dB["out"]), [])
        P.emit()
    return nc


_NC_CACHE = {}


def make_in_maps(cfg, inp):
    NT = cfg.NT
    x = np.ascontiguousarray(inp["x"], dtype=np.float32)
    ctx = np.asarray(inp["ctx"], np.float32)
    maps = []
    shared = {
        "c_ctx": np.ascontiguousarray(inp["c_ctx"], np.float32).reshape(-1),
        "w_mod": np.ascontiguousarray(inp["w_mod"][0], np.float32),
        "b_mod": np.ascontiguousarray(inp["b_mod"][0], np.float32).reshape(-1),
        "norm_pre": np.ascontiguousarray(inp["norm_pre"][0], np.float32).reshape(-1),
        "norm_post": np.ascontiguousarray(inp["norm_post"][0], np.float32).reshape(-1),
        "ffn_w_in": np.ascontiguousarray(inp["ffn_w_in"][0], np.float32),
        "ffn_w_out": np.ascontiguousarray(inp["ffn_w_out"][0], np.float32),
        "w_in": np.ascontiguousarray(inp["w_in"][0], np.float32),
        "w_out": np.ascontiguousarray(inp["w_out"][0], np.float32),
        "conv_w": np.ascontiguousarray(inp["conv_w"][0], np.float32).reshape(-1),
        "conv_b": np.ascontiguousarray(inp["conv_b"][0], np.float32).reshape(-1),
        "w_q": np.ascontiguousarray(inp["w_q"][0], np.float32),
        "w_k": np.ascontiguousarray(inp["w_k"][0], np.float32),
        "gbias": np.concatenate([np.asarray(inp["i_bias"][0], np.float32).reshape(-1),
                                 np.asarray(inp["f_bias"][0], np.float32).reshape(-1)]),
        "head_norm": np.ascontiguousarray(inp["head_norm"][0], np.float32).reshape(-1),
        "pool_w": np.ascontiguousarray(inp["pool_w"][0], np.float32),
        "pool_scale": np.ascontiguousarray(inp["pool_scale"][0], np.float32).reshape(-1),
    }
    for core in range(NCORES):
        b, r = core // 4, core % 4
        t0 = r * NT * 512
        m = dict(shared)
        m["x_loc"] = np.ascontiguousarray(x[b, t0:t0 + NT * 512])
        aux = np.zeros((cfg.NA, cfg.D), np.float32)
        aux[0:cfg.CTX] = ctx[b]
        if r > 0:
            aux[cfg.CTX] = x[b, t0 - 1]
        if r < 3:
            aux[cfg.CTX + 1] = x[b, t0 + NT * 512]
        m["x_aux"] = aux
        m["c_loc"] = np.ascontiguousarray(inp["c"][b], np.float32).reshape(-1)
        m.update(host_tables(cfg, core))
        maps.append(m)
    return maps


def run_cfg(cfg, inp):
    key = (cfg.D, cfg.DFF, cfg.NT, cfg.CTX)
    if key not in _NC_CACHE:
        _NC_CACHE[key] = build(cfg)
    nc = _NC_CACHE[key]
    maps = make_in_maps(cfg, inp)
    res = run_bass_kernel_spmd(nc, maps, core_ids=list(range(NCORES)))
    NT = cfg.NT
    out = np.zeros((2, 4 * NT * 512, cfg.D), np.float32)
    for core in range(NCORES):
        b, r = core // 4, core % 4
        out[b, r * NT * 512:(r + 1) * NT * 512] = res.results[core]["out"]
    return out


def kernel(**inputs):
    cfg = Cfg()
    return run_cfg(cfg, inputs)
```

```python
import os
import numpy as np
from contextlib import ExitStack
import concourse.bass as bass
import concourse.mybir as mybir
from concourse.bass_utils import run_bass_kernel_spmd

F32 = mybir.dt.float32
BF16 = mybir.dt.bfloat16
AF = mybir.ActivationFunctionType
ALU = mybir.AluOpType
AX = mybir.AxisListType
SEM_CAP = 20000
KSTOP = float(os.environ.get('KSTOP', '99'))
KOFF = os.environ.get('KOFF', '').split(',')
ACCT_ONLY = False
NCORES = 8


class Buf:
    __slots__ = ("w", "r")

    def __init__(self):
        self.w = None
        self.r = []


class DGroup:
    __slots__ = ("key", "final")

    def __init__(self, key):
        self.key = key
        self.final = 0


class Op:
    __slots__ = ("eng", "fn", "deps", "signal", "sig", "grp", "dval", "isdma")

    def __init__(self, eng, fn):
        self.eng = eng
        self.fn = fn
        self.deps = []
        self.signal = False
        self.sig = 0
        self.grp = None
        self.dval = 0
        self.isdma = False


class Prog:
    ENGS = ("pe", "act", "dve", "pool", "sp")

    def __init__(self, nc):
        self.nc = nc
        self.ops = {e: [] for e in self.ENGS}
        self.dcount = {}
        self.dlast = {}

    def _deps(self, op, reads, writes):
        deps = []
        for b in reads:
            if b.w is not None:
                deps.append(("raw", b.w))
            b.r.append(op)
        for b in writes:
            if b.w is not None:
                deps.append(("waw", b.w))
            for r in b.r:
                if r is not op:
                    deps.append(("war", r))
            b.w = op
            b.r = []
        out = []
        seen = set()
        for kind, d in deps:
            if d is op or id(d) in seen:
                continue
            if (not d.isdma) and (not op.isdma) and d.eng == op.eng:
                if op.eng == "pe":
                    continue
            seen.add(id(d))
            out.append(d)
        op.deps = out
        for d in out:
            d.signal = True

    def op(self, eng, fn, reads=(), writes=()):
        o = Op(eng, fn)
        self._deps(o, list(reads), list(writes))
        self.ops[eng].append(o)
        return o

    def dma(self, eng, out, in_, reads=(), writes=(), sem=None, join=False, **kw):
        o = Op(eng, lambda e: e.dma_start(out=out, in_=in_, **kw))
        o.isdma = True
        key = sem
        if join and key in self.dlast:
            group = self.dlast[key]
            prev = None
        else:
            group = DGroup(key)
            prev = self.dlast.get(key)
            self.dlast[key] = group
        o.grp = group
        self._deps(o, list(reads), list(writes))
        o.deps = [d for d in o.deps if not (isinstance(d, Op) and d.grp is group and d is not o)]
        if prev is not None:
            o.deps.append(prev)
        self.dcount[key] = self.dcount.get(key, 0) + 16
        group.final = self.dcount[key]
        o.signal = True
        self.ops[eng].append(o)
        return o

    def cc(self, fn, reads=(), writes=(), sem=None):
        o = Op("pool", fn)
        o.isdma = True
        group = DGroup(sem)
        assert sem not in self.dcount
        self.dcount[sem] = 1
        group.final = 1
        self.dlast[sem] = group
        o.grp = group
        self._deps(o, list(reads), list(writes))
        o.signal = True
        o.dval = -1
        self.ops["pool"].append(o)
        return o

    def barrier(self, tiny):
        a = {}
        for e in ("pe", "act", "dve", "pool"):
            o = Op(e, tiny[e])
            o.signal = True
            self.ops[e].append(o)
            a[e] = o
        groups = [g for g in self.dlast.values()]
        for e in self.ENGS:
            o = Op(e, None)
            o.deps = [a[x] for x in a if x != e] + groups
            self.ops[e].append(o)

    def emit(self):
        nc = self.nc
        for e in self.ENGS:
            n = 0
            for o in self.ops[e]:
                if o.isdma:
                    continue
                if o.signal:
                    n += 1
                    o.sig = n
        nsem = {e: max(1, -(-max([o.sig for o in self.ops[e] if not o.isdma] + [0]) // SEM_CAP)) for e in self.ENGS}
        with ExitStack() as st:
            esems = {e: [st.enter_context(nc.semaphore(f"s_{e}{i}")) for i in range(nsem[e])] for e in self.ENGS}
            dsems = {k: st.enter_context(nc.semaphore(f"d_{i}")) for i, k in enumerate(self.dcount)}
            block = st.enter_context(nc.Block())

            def run(ename, eng):
                waited = {}
                for o in self.ops[ename]:
                    for d in o.deps:
                        if isinstance(d, DGroup):
                            s, v = dsems[d.key], d.final
                        elif d.isdma:
                            s, v = dsems[d.grp.key], d.grp.final
                        else:
                            k = (d.sig - 1) // SEM_CAP
                            s, v = esems[d.eng][k], d.sig - k * SEM_CAP
                        sid = id(s)
                        if waited.get(sid, 0) >= v:
                            continue
                        waited[sid] = v
                        eng.wait_ge(s, v)
                    if o.fn is None:
                        continue
                    ins = o.fn(eng)
                    if o.isdma and o.dval == -1:
                        ins.then_inc(dsems[o.grp.key])
                    elif o.isdma:
                        ins.then_inc(dsems[o.grp.key], 16)
                    elif o.signal:
                        k = (o.sig - 1) // SEM_CAP
                        ins.then_inc(esems[ename][k], 1)

            @block.tensor
            def _(e):
                run("pe", e)

            @block.scalar
            def _(e):
                run("act", e)

            @block.vector
            def _(e):
                run("dve", e)

            @block.gpsimd
            def _(e):
                run("pool", e)

            @block.sync
            def _(e):
                run("sp", e)


class Cfg:
    def __init__(self, D=1024, DFF=2816, NT=8, CTX=256):
        self.D, self.DFF, self.NT, self.CTX = D, DFF, NT, CTX
        self.KC, self.JC = D // 128, DFF // 128
        self.CC = CTX // 128
        self.NA = CTX + 128
        self.NB = NT * 4
        self.INC = 2064
        self.SEQ = 4 * NT * 512
        self.ROWS = self.SEQ // 64


POOL_WINDOWS = (2, 4, 8, 16)


def pool_offsets():
    offs = []
    for g, w in enumerate(POOL_WINDOWS):
        lo, hi = w // 2, w - 1 - w // 2
        for dl in range(-((lo + 1) // 2), (1 + hi) // 2 + 1):
            offs.append((g, dl))
    return offs


def host_tables(cfg, core):
    b, r = core // 4, core % 4
    t = {}
    fl = np.array([r == 0, r == 3, r > 0, r < 3], np.float32)
    t["flags"] = np.tile(fl[None, :], (128, 1)).astype(np.float32)
    pred = np.zeros((2, 8), np.float32)
    mbt = np.zeros((2, 8, 8), np.float32)
    for q in range(8):
        if q // 4 != b:
            continue
        if q < core:
            pred[0, q] = 1
            for q2 in range(q + 1, core):
                mbt[0, q, q2] = 1
        if q > core:
            pred[1, q] = 1
            for q2 in range(core + 1, q):
                mbt[1, q, q2] = 1
    predt = np.zeros((8, 8), np.float32)
    for q in range(8):
        predt[q, 0:4] = pred[0, q]
        predt[q, 4:8] = pred[1, q]
    t["pred"] = np.tile(predt.reshape(1, 64), (128, 1)).astype(np.float32)
    t["mbt"] = np.tile(mbt.reshape(1, 128), (128, 1)).astype(np.float32)
    sel = np.zeros((2, 8), np.float32)
    if r > 0:
        sel[0, core - 1] = 1
    if r < 3:
        sel[1, core + 1] = 1
    t["sel"] = np.tile(sel.reshape(1, 16), (128, 1)).astype(np.float32)
    R = cfg.ROWS
    inv = np.zeros((128, cfg.NB, 4), np.float32)
    for blk in range(cfg.NB):
        for p in range(128):
            row = r * cfg.NT * 8 + blk * 2 + p // 64
            col = p % 64
            for g, w in enumerate(POOL_WINDOWS):
                lo, hi = w // 2, w - 1 - w // 2
                cr = min(row + hi, R - 1) - max(row - lo, 0) + 1
                cc = min(col + hi, 63) - max(col - lo, 0) + 1
                inv[p, blk, g] = 1.0 / (cr * cc)
    t["invcnt"] = inv.reshape(128, cfg.NB * 4)
    offs = pool_offsets()
    pm = np.zeros((128, len(offs), 128), np.float32)
    tin = np.arange(128)
    for i, (g, dl) in enumerate(offs):
        w = POOL_WINDOWS[g]
        lo, hi = w // 2, w - 1 - w // 2
        rin = 2 * dl + tin // 64
        cin = tin % 64
        for to in range(128):
            ro, co = to // 64, to % 64
            ok = (rin >= ro - lo) & (rin <= ro + hi) & (cin >= co - lo) & (cin <= co + hi)
            pm[:, i, to] = ok.astype(np.float32)
    t["pm"] = pm.reshape(128, len(offs) * 128)
    return t


class T:
    def __init__(self, t, nb=1):
        self.t = t
        self.b = [Buf() for _ in range(nb)]

    @property
    def B(self):
        return self.b


def build(cfg):
    D, DFF, NT, KC, JC, CC, NA, NB = cfg.D, cfg.DFF, cfg.NT, cfg.KC, cfg.JC, cfg.CC, cfg.NA, cfg.NB
    EPS = 1e-6
    nc = bass.Bass("TRN2", target_bir_lowering=False)
    P = Prog(nc)
    offs = pool_offsets()
    NOFF = len(offs)

    def din(name, shape, dt=F32):
        return nc.dram_tensor(name, list(shape), dt, kind="ExternalInput")

    x_loc = din("x_loc", [NT * 512, D])
    x_aux = din("x_aux", [NA, D])
    c_loc = din("c_loc", [D])
    c_ctx = din("c_ctx", [D])
    w_mod = din("w_mod", [D, 9 * D])
    b_mod = din("b_mod", [9 * D])
    norm_pre = din("norm_pre", [3 * D])
    norm_post = din("norm_post", [3 * D])
    ffn_w_in = din("ffn_w_in", [2, D, 2 * DFF])
    ffn_w_out = din("ffn_w_out", [2, DFF, D])
    w_in = din("w_in", [D, cfg.INC])
    w_out = din("w_out", [1024, D])
    conv_w = din("conv_w", [3 * 512])
    conv_b = din("conv_b", [512])
    w_q = din("w_q", [4, 128, 128])
    w_k = din("w_k", [4, 128, 128])
    gbias = din("gbias", [16])
    head_norm = din("head_norm", [512])
    pool_w = din("pool_w", [4, 128, 128])
    pool_scale = din("pool_scale", [512])
    flags_d = din("flags", [128, 4])
    pred_d = din("pred", [128, 64])
    mbt_d = din("mbt", [128, 128])
    sel_d = din("sel", [128, 16])
    invcnt_d = din("invcnt", [128, NB * 4])
    pm_d = din("pm", [128, NOFF * 128])
    out_d = nc.dram_tensor("out", [NT * 512, D], F32, kind="ExternalOutput")

    Win_s = nc.dram_tensor("Win_s", [2 * JC, 128, KC * 256], BF16)
    Wout_s = nc.dram_tensor("Wout_s", [2 * KC, 128, JC * 128], BF16)
    Wi_s = nc.dram_tensor("Wi_s", [128, KC * cfg.INC], BF16)
    Wo_s = nc.dram_tensor("Wo_s", [128, 8 * D], BF16)
    X1_s = nc.dram_tensor("X1_s", [NT, 128, KC * 512], F32)
    QT_s = nc.dram_tensor("QT_s", [NT, 128, 2048], BF16)
    KT_s = nc.dram_tensor("KT_s", [NT, 128, 2048], BF16)
    KTOK_s = nc.dram_tensor("KTOK_s", [NB, 128, 512], BF16)
    V_s = nc.dram_tensor("V_s", [NB, 128, 4 * 129], BF16)
    O_s = nc.dram_tensor("O_s", [NB, 128, 512], F32)
    PL_s = nc.dram_tensor("PL_s", [NB, 128, 512], F32)
    PLb_s = nc.dram_tensor("PLb_s", [NB, 128, 512], BF16)
    SNAP_s = nc.dram_tensor("SNAP_s", [NB, 128, 4 * 129], BF16)
    CCW = 1040
    cc1_in = nc.dram_tensor("cc1_in", [128, CCW], F32)
    cc1_out = nc.dram_tensor("cc1_out", [128 * NCORES, CCW], F32)
    cc2_in = nc.dram_tensor("cc2_in", [128, 4096], BF16)
    cc2_out = nc.dram_tensor("cc2_out", [128 * NCORES, 4096], BF16)
    dB = {k: [Buf() for _ in range(n)] for k, n in dict(Win=2 * JC, Wout=2 * KC, Wi=1, Wo=1, X1=NT, QT=NT, KT=NT,
                                                         KTOK=NB, V=NB, O=NB, PL=NB, PLb=NB, SNAP=NB, cc1i=1, cc1o=1,
                                                         cc2i=1, cc2o=1, out=1).items()}

    st = ExitStack()
    with st:
        acct = {}

        def sb(name, shape, dt=F32, nb=1):
            n = 1
            for v in shape[1:]:
                n *= v
            acct[name] = -(-(n * (2 if dt == BF16 else 4)) // 32) * 32
            if ACCT_ONLY:
                return T(None, nb)
            return T(st.enter_context(nc.sbuf_tensor(name, list(shape), dt)), nb)

        def al(base, off, shape, dt, nb=None):
            n = 1
            for v in shape[1:]:
                n *= v
            nbytes = n * (2 if dt == BF16 else 4)
            t = T(None, 0)
            t.b = list(base.b) if nb is None else [base.b[0]] * nb
            if ACCT_ONLY:
                return t
            bt = base.t
            flat = bt[:] if len(bt.shape) == 2 else (bt[:].rearrange("p a b -> p (a b)") if len(bt.shape) == 3 else
                                                     bt[:].rearrange("p a b c -> p (a b c)"))
            esz = 2 if flat.dtype == BF16 else 4
            assert off % 32 == 0 and off % esz == 0 and (off + nbytes) <= flat.shape[1] * esz, (off, nbytes, flat.shape)
            v = flat[:, off // esz:(off + nbytes + esz - 1) // esz]
            if flat.dtype != dt:
                v = v.bitcast(dt)
            v = v[:, 0:n]
            if len(shape) == 3:
                v = v.rearrange("p (a b) -> p a b", a=shape[1])
            elif len(shape) == 4:
                v = v.rearrange("p (a b c) -> p a b c", a=shape[1], b=shape[2])
            t.t = v
            return t

        identF = sb("identF", [128, 128]); identB = sb("identB", [128, 128], BF16)
        onesF = sb("onesF", [128, 128]); onesB = sb("onesB", [128, 128], BF16)
        triF = sb("triF", [128, 128]); triB = sb("triB", [128, 128])
        tiny = sb("tiny", [128, 8], F32, nb=4)
        NV = 2 * KC + 9 * KC + 3 * KC + 3 * KC + 12 + 4 + 4
        VT = sb("VT", [128, NV])
        SC = sb("SC", [128, KC, 2])
        MODT = sb("MODT", [128, 9 * KC, 2])
        AIN = sb("AIN", [128, 3, KC, 2]); AOUT = sb("AOUT", [128, 3, KC, 2])
        flags = sb("flags_sb", [128, 4]); predt = sb("pred_sb", [128, 8, 8]); mbt = sb("mbt_sb", [128, 2, 8, 8])
        sel = sb("sel_sb", [128, 2, 8]); invcnt = sb("invcnt_sb", [128, NB, 4])
        PM = sb("PM", [128, NOFF, 128], BF16)
        HNORM = sb("HNORM", [128, 512]); GBIAS = sb("GBIAS", [128, 16])
        wq = sb("wq", [128, 4, 128], BF16); wk = sb("wk", [128, 4, 128], BF16); pw = sb("pw", [128, 4, 128], BF16)
        BIG0 = sb("BIG0", [128, 4096], F32, nb=8)
        BIG1 = sb("BIG1", [128, 4096], F32, nb=8)
        yT = sb("yT", [128, KC, 512], BF16, nb=KC)
        hT = sb("hT", [128, max(JC * 512, 11264)], BF16, nb=JC)
        sq = [sb(f"sq{i}", [128, 512], BF16) for i in range(2)]
        rstd = sb("rstd", [128, 512])
        tmpf = [sb(f"tmpf{i}", [128, 512]) for i in range(3)]
        WIN = [sb(f"WIN{i}", [128, KC, 256], BF16) for i in range(3)]
        WOUT = [sb(f"WOUT{i}", [128, JC, 128], BF16) for i in range(2)]
        XIO = [sb(f"XIO{i}", [128, D]) for i in range(2)]
        rowsA = al(XIO[0], 0, [128, 128], F32); rowsB = al(XIO[1], 0, [128, 128], F32)
        QT = sb("QT", [128, 4, 512], BF16); KT = sb("KT", [128, 4, 512], BF16)
        KTOK = [sb(f"KTOK{i}", [128, 4, 128], BF16) for i in range(2)]
        VXT = [sb(f"VXT{i}", [128, 4, 129], BF16) for i in range(8)]
        OT = [sb(f"OT{i}", [128, 512]) for i in range(2)]
        PT = [sb(f"PT{i}", [128, 512]) for i in range(2)]
        PTb = [sb(f"PTb{i}", [128, 512], BF16) for i in range(2)]
        VW = [sb(f"VW{i}", [128, 4, 129], BF16) for i in range(2)]
        NCH = NB + CC
        GP = sb("GP", [128, NCH, 32], F32, nb=NCH)
        GPX = sb("GPX", [128, NCH, 4], F32, nb=NCH)
        GA = sb("GA", [128, 16]); LF = sb("LF", [128, 8]); G8 = sb("G8", [128, 8])
        C_b = sb("C_b", [128, 4, 129]); T_f = sb("T_f", [128, 4, 129])
        C_f = sb("C_f", [128, 4, 129]); SINB = sb("SINB", [128, 4, 129])
        FCTX = SINB
        TMPS = T_f
        ESF = sb("ESF", [128, 4]); ESB = sb("ESB", [128, 4]); GT = sb("GT", [128, 8]); WC = sb("WC", [128, 4]); WCB = sb("WCB", [128, 4])
        SNP = [sb(f"SNP{i}", [128, 4, 129], BF16) for i in range(2)]
        RT = [sb(f"RT{i}", [128, 2, 129]) for i in range(2)]
        PQ = [sb(f"PQ{i}", [128, 4, 514]) for i in range(2)]
        uT = sb("uT", [128, 4, 512], BF16)
        HL = sb("HL", [128, 4]); HR = sb("HR", [128, 4])
        WPC = [sb(f"WPC{i}", [128, KC, 512], BF16) for i in range(2)]
        WG = sb("WG", [128, KC, 16], BF16)
        MIXT = al(PQ[0], 0, [128, 8, 512], BF16, nb=8)
        HALO = al(PQ[1], 0, [128, 8, 512], BF16)
        RING = [al(hT, i * 1024, [128, 512], BF16) for i in range(10)]
        HS = al(hT, 10240, [128, 4, 128], F32); HSQ = al(hT, 12288, [128, 4, 128], F32); SG = al(hT, 14336, [128, 512], F32)
        SM = [al(hT, 16384 + i * 1024, [128, 4, 128], BF16) for i in range(2)]
        ND = [al(hT, 18432 + i * 1056, [128, 2, 129], F32) for i in range(2)]
        MO = al(hT, 20544, [128, 4, 128], BF16)
        DT_ = al(uT, 0, [128, 4, 128], BF16); DTT = al(uT, 1024, [128, 4, 128], BF16)
        STR = [al(BIG0, i * 4160, [128, CCW], F32) for i in range(2)]
        CFB = al(BIG0, 8320, [128, 4, 129], BF16); CBC = al(BIG0, 9376, [128, 4, 129], BF16)
        HQ = [al(hT, i * 8192, [128, 8, 512], BF16) for i in range(2)]
        SM8 = [sb(f"SM8_{i}", [128, 8]) for i in range(4)]
        GTQ = sb("GTQ", [128, 8, 8]); GTT = sb("GTT", [128, 8, 8]); ARGS = sb("ARGS", [128, 8, 8]); WQ8 = sb("WQ8", [128, 8, 8])
        TM48 = sb("TM48", [128, 4, 8])

        if ACCT_ONLY:
            tot = sum(acct.values())
            print("SBUF bytes/partition:", tot, "limit", nc.sbuf_top - nc.sbuf_base)
            for k, v in sorted(acct.items(), key=lambda kv: -kv[1])[:40]:
                print("  ", k, v)
            return None
        PS = [T(st.enter_context(nc.psum_tensor(f"ps{i}", [128, 512], F32))) for i in range(7)]
        PSB = T(st.enter_context(nc.psum_tensor("psb", [128, 8, 128], BF16)))
        psi = [0]

        def nps():
            p = PS[psi[0] % 7]
            psi[0] += 1
            return p

        def bl(*ts):
            out = []
            for t in ts:
                if isinstance(t, T):
                    out += t.b
                elif isinstance(t, Buf):
                    out.append(t)
                else:
                    out += list(t)
            return out

        def act(out, in_, func, r, w, bias=None, scale=None):
            kw = {}
            if bias is not None:
                kw["bias"] = bias
            if scale is not None:
                kw["scale"] = scale
            P.op("act", lambda e: e.activation(out, in_, func, **kw), bl(*r), bl(*w))

        def cp(eng, out, in_, r, w):
            if eng == "act":
                P.op("act", lambda e: e.copy(out, in_), bl(*r), bl(*w))
            else:
                P.op(eng, lambda e: e.tensor_copy(out, in_), bl(*r), bl(*w))

        def tt(eng, out, in0, in1, op, r, w):
            P.op(eng, lambda e: e.tensor_tensor(out, in0, in1, op), bl(*r), bl(*w))

        def ts(eng, out, in0, s1, s2, op0, op1, r, w):
            P.op(eng, lambda e: e.tensor_scalar(out, in0, s1, s2, op0, op1), bl(*r), bl(*w))

        def ts1(eng, out, in0, s1, op0, r, w):
            P.op(eng, lambda e: e.tensor_single_scalar(out, in0, s1, op0), bl(*r), bl(*w))

        def stt(eng, out, in0, sc, in1, op0, op1, r, w):
            P.op(eng, lambda e: e.scalar_tensor_tensor(out, in0, sc, in1, op0, op1), bl(*r), bl(*w))

        def mm(out, lhsT, rhs, start, stop, r, w):
            P.op("pe", lambda e: e.matmul(out, lhsT, rhs, start=start, stop=stop), bl(*r), bl(*w))

        def tr(out, in_, ident, r, w):
            P.op("pe", lambda e: e.transpose(out, in_, ident), bl(*r), bl(*w))

        def dma(out, in_, r, w, sem, join=False):
            P.dma("sp", out, in_, bl(*r), bl(*w), sem=sem, join=join)

        def recip(out, in_, r, w):
            P.op("dve", lambda e: e.reciprocal(out, in_), bl(*r), bl(*w))

        def memset(eng, ap, val, w):
            P.op(eng, lambda e: e.memset(ap, val), [], bl(*w))

        memset("pool", identF.t[:], 1.0, [identF])
        P.op("pool", lambda e: e.affine_select(out=identF.t[:], in_=identF.t[:], pattern=[[-1, 128]],
                                               compare_op=ALU.is_equal, fill=0.0, base=0, channel_multiplier=1),
             identF.b, identF.b)
        cp("pool", identB.t[:], identF.t[:], [identF], [identB])
        memset("pool", onesF.t[:], 1.0, [onesF]); memset("pool", onesB.t[:], 1.0, [onesB])
        memset("pool", triF.t[:], 1.0, [triF]); memset("pool", triB.t[:], 1.0, [triB])
        P.op("pool", lambda e: e.affine_select(out=triF.t[:], in_=triF.t[:], pattern=[[1, 128]], compare_op=ALU.is_ge,
                                               fill=0.0, base=0, channel_multiplier=-1), triF.b, triF.b)
        P.op("pool", lambda e: e.affine_select(out=triB.t[:], in_=triB.t[:], pattern=[[-1, 128]], compare_op=ALU.is_ge,
                                               fill=0.0, base=0, channel_multiplier=1), triB.b, triB.b)
        for v in VXT:
            memset("pool", v.t[:, :, 128:129], 1.0, [v])

        dma(flags.t[:], flags_d[:, :], [], [flags], "tb")
        dma(predt.t[:].rearrange("p a b -> p (a b)"), pred_d[:, :], [], [predt], "tb", True)
        dma(mbt.t[:].rearrange("p d a b -> p (d a b)"), mbt_d[:, :], [], [mbt], "tb", True)
        dma(sel.t[:].rearrange("p a b -> p (a b)"), sel_d[:, :], [], [sel], "tb", True)
        dma(invcnt.t[:].rearrange("p a b -> p (a b)"), invcnt_d[:, :], [], [invcnt], "tb", True)
        dma(HNORM.t[:], head_norm.ap().partition_broadcast(128), [], [HNORM], "tb", True)
        dma(GBIAS.t[:], gbias.ap().partition_broadcast(128), [], [GBIAS], "tb", True)
        b0v = BIG0.t[:, 0:NOFF * 128]
        dma(b0v, pm_d[:, :], [], [BIG0], "stg0")
        cp("pool", PM.t[:].rearrange("p a b -> p (a b)"), b0v, [BIG0], [PM])

        vecs = [("c", c_loc, KC), ("cx", c_ctx, KC), ("bm", b_mod, 9 * KC), ("npre", norm_pre, 3 * KC),
                ("npost", norm_post, 3 * KC), ("cw", conv_w, 12), ("cb", conv_b, 4), ("psc", pool_scale, 4)]
        voff = {}
        r0 = 0
        memset("pool", rowsA.t[:], 0.0, [rowsA]); memset("pool", rowsB.t[:], 0.0, [rowsB])
        for name, dten, n in vecs:
            voff[name] = r0
            src = dten.ap().rearrange("(r c) -> r c", c=128)
            a, bnd = r0, r0 + n
            if a < 128:
                e_ = min(bnd, 128)
                dma(rowsA.t[a:e_, :], src[0:e_ - a, :], [], [rowsA], "tb", True)
            if bnd > 128:
                s_ = max(a, 128)
                dma(rowsB.t[s_ - 128:bnd - 128, :], src[s_ - a:n, :], [], [rowsB], "tb", True)
            r0 += n
        assert r0 == NV and NV <= 256
        p_ = nps()
        tr(p_.t[:, 0:128], rowsA.t[:], identF.t[:], [rowsA, identF], [p_])
        na = min(NV, 128)
        cp("dve", VT.t[:, 0:na], p_.t[:, 0:na], [p_], [VT])
        if NV > 128:
            nb_ = NV - 128
            p_ = nps()
            tr(p_.t[:, 0:nb_], rowsB.t[0:nb_, :], identF.t[0:nb_, 0:nb_], [rowsB, identF], [p_])
            cp("dve", VT.t[:, 128:NV], p_.t[:, 0:nb_], [p_], [VT])

        def vcol(name, i):
            c = voff[name] + i
            return VT.t[:, c:c + 1]

        act(SC.t[:, :, 0], VT.t[:, voff["c"]:voff["c"] + KC], AF.Silu, [VT], [SC])
        act(SC.t[:, :, 1], VT.t[:, voff["cx"]:voff["cx"] + KC], AF.Silu, [VT], [SC])

        if KSTOP <= 1:
            P.emit()
            return nc
        stg = [BIG0, BIG1]
        cast_i = [0]

        def cast(out, in_, r, w):
            e = ("act", "act", "pool")[cast_i[0] % 3]
            cast_i[0] += 1
            if e == "dve":
                ts1("dve", out, in_, 1.0, ALU.mult, r, w)
            else:
                cp(e, out, in_, r, w)
        cst = [hT.t[:, 0:2816], hT.t[:, 2816:5632]]
        cstB = [hT.b[0:max(1, JC // 2)], hT.b[max(1, JC // 2):]]
        wi_ = [0]

        def prep(src_aps, width, dst_ap, dstbuf):
            i = wi_[0] % 2
            wi_[0] += 1
            o = 0
            for k, (sap, wd) in enumerate(src_aps):
                dma(stg[i].t[:, o:o + wd] if len(sap.shape) == 2 else stg[i].t[:, o:o + wd].rearrange(
                    "p (a b) -> p a b", a=sap.shape[1]), sap, [], [stg[i]], f"stg{i}", k > 0)
                o += wd
            assert o == width
            cast(cst[i][:, 0:width], stg[i].t[:, 0:width], [stg[i]], cstB[i])
            dma(dst_ap, cst[i][:, 0:width], cstB[i], [dstbuf], f"cso{i}")

        for f in range(2):
            for j in range(JC):
                src = ffn_w_in[f].rearrange("(kc p) c -> p kc c", p=128)
                i = wi_[0] % 2
                wi_[0] += 1
                sv = stg[i].t[:, 0:KC * 256].rearrange("p (kc c) -> p kc c", kc=KC)
                dma(sv[:, :, 0:128], src[:, :, j * 128:(j + 1) * 128], [], [stg[i]], f"stg{i}")
                dma(sv[:, :, 128:256], src[:, :, DFF + j * 128:DFF + (j + 1) * 128], [], [stg[i]], f"stg{i}", True)
                cast(cst[i][:, 0:KC * 256], stg[i].t[:, 0:KC * 256], [stg[i]], cstB[i])
                dma(Win_s[f * JC + j], cst[i][:, 0:KC * 256], cstB[i], [dB["Win"][f * JC + j]], f"cso{i}")
            for m in range(KC):
                src = ffn_w_out[f].rearrange("(jc p) c -> p jc c", p=128)[:, :, m * 128:(m + 1) * 128]
                prep([(src, JC * 128)], JC * 128, Wout_s[f * KC + m], dB["Wout"][f * KC + m])
        wsrc = w_in.ap().rearrange("(kc p) c -> p kc c", p=128)
        wdst = Wi_s.ap().rearrange("p (kc c) -> p kc c", kc=KC)
        c0 = 0
        while c0 < cfg.INC:
            cw_ = min(256, cfg.INC - c0)
            i = wi_[0] % 2
            wi_[0] += 1
            sv = stg[i].t[:, 0:KC * cw_].rearrange("p (kc c) -> p kc c", kc=KC)
            cv = cst[i][:, 0:KC * cw_].rearrange("p (kc c) -> p kc c", kc=KC)
            dma(sv, wsrc[:, :, c0:c0 + cw_], [], [stg[i]], f"stg{i}")
            cast(cst[i][:, 0:KC * cw_], stg[i].t[:, 0:KC * cw_], [stg[i]], cstB[i])
            dma(wdst[:, :, c0:c0 + cw_], cv, cstB[i], [dB["Wi"][0]], f"cso{i}")
            c0 += cw_
        wsrc = w_out.ap().rearrange("(mc p) c -> p mc c", p=128)
        wdst = Wo_s.ap().rearrange("p (mc c) -> p mc c", mc=8)
        for c0 in range(0, D, 256):
            i = wi_[0] % 2
            wi_[0] += 1
            sv = stg[i].t[:, 0:8 * 256].rearrange("p (kc c) -> p kc c", kc=8)
            cv = cst[i][:, 0:8 * 256].rearrange("p (kc c) -> p kc c", kc=8)
            dma(sv, wsrc[:, :, c0:c0 + 256], [], [stg[i]], f"stg{i}")
            cast(cst[i][:, 0:2048], stg[i].t[:, 0:2048], [stg[i]], cstB[i])
            dma(wdst[:, :, c0:c0 + 256], cv, cstB[i], [dB["Wo"][0]], f"cso{i}")
        for dst, srcw in ((wq, w_q), (wk, w_k), (pw, pool_w)):
            i = wi_[0] % 2
            wi_[0] += 1
            sv = stg[i].t[:, 0:512].rearrange("p (h c) -> p h c", h=4)
            dma(sv, srcw.ap().rearrange("h p c -> p h c"), [], [stg[i]], f"stg{i}")
            cp("pool", dst.t[:], sv, [stg[i]], [dst])

        if KSTOP <= 2:
            P.emit()
            return nc
        pm_ = nps()
        pmv = pm_.t[:, 0:18 * KC].rearrange("p (a b) -> p a b", b=2)
        gw = 4 if (9 * KC) % 4 == 0 else 2
        wmsrc = w_mod.ap().rearrange("(kc p) c -> p kc c", p=128)
        for og in range(9 * KC // gw):
            i = wi_[0] % 2
            wi_[0] += 1
            sv = stg[i].t[:, 0:KC * gw * 128].rearrange("p (kc c) -> p kc c", kc=KC)
            dma(sv, wmsrc[:, :, og * gw * 128:(og + 1) * gw * 128], [], [stg[i]], f"stg{i}")
            for o4 in range(gw):
                oc = og * gw + o4
                for kc in range(KC):
                    mm(pmv[:, oc, :], sv[:, kc, o4 * 128:(o4 + 1) * 128], SC.t[:, kc, :], kc == 0, kc == KC - 1,
                       [stg[i], SC], [pm_])
        bm0 = voff["bm"]
        tt("dve", MODT.t[:], pmv, VT.t[:, bm0:bm0 + 9 * KC].unsqueeze(2).to_broadcast([128, 9 * KC, 2]), ALU.add,
           [pm_, VT], [MODT])
        RESW = (0.5, 1.0, 0.5)
        for j in range(3):
            gpre = VT.t[:, voff["npre"] + j * KC: voff["npre"] + (j + 1) * KC].unsqueeze(2).to_broadcast([128, KC, 2])
            gpost = VT.t[:, voff["npost"] + j * KC: voff["npost"] + (j + 1) * KC].unsqueeze(2).to_broadcast([128, KC, 2])
            ts1("dve", AIN.t[:, j], MODT.t[:, (3 * j + 1) * KC:(3 * j + 2) * KC, :], 1.0, ALU.add, [MODT], [AIN])
            tt("dve", AIN.t[:, j], AIN.t[:, j], gpre, ALU.mult, [AIN, VT], [AIN])
            tt("dve", AOUT.t[:, j], MODT.t[:, (3 * j + 2) * KC:(3 * j + 3) * KC, :], gpost, ALU.mult, [MODT, VT], [AOUT])
            ts1("dve", AOUT.t[:, j], AOUT.t[:, j], RESW[j], ALU.mult, [AOUT], [AOUT])

        def ain(j, kc, mi):
            return AIN.t[:, j, kc, mi:mi + 1]

        def bin_(j, kc, mi):
            return MODT.t[:, 3 * j * KC + kc, mi:mi + 1]

        def aout(j, kc, mi):
            return AOUT.t[:, j, kc, mi:mi + 1]

        if KSTOP <= 3:
            P.emit()
            return nc
        xT = BIG1.t[:, 0:KC * 512].rearrange("p (kc n) -> p kc n", kc=KC)
        zT = BIG0.t[:, 0:KC * 512].rearrange("p (kc n) -> p kc n", kc=KC)
        xTb, zTb = BIG1.b, BIG0.b
        hTv = hT.t[:, 0:JC * 512].rearrange("p (j n) -> p j n", j=JC)

        def load_xT(src_rows, N):
            for bi in range(N // 128):
                xi = XIO[bi % 2]
                dma(xi.t[:], src_rows[bi * 128:(bi + 1) * 128, :], [], [xi], f"xin{bi % 2}")
                for k0 in range(0, KC, 4):
                    kn = min(4, KC - k0)
                    p = nps()
                    for k in range(kn):
                        tr(p.t[:, k * 128:(k + 1) * 128], xi.t[:, (k0 + k) * 128:(k0 + k + 1) * 128], identF.t[:],
                           [xi, identF], [p])
                    cp("dve", xT[:, k0:k0 + kn, bi * 128:(bi + 1) * 128],
                       p.t[:, 0:kn * 128].rearrange("p (k n) -> p k n", k=kn), [p], xTb[k0:k0 + kn])

        def stats(src, srcb, N):
            p = nps()
            for kc in range(KC):
                s = sq[kc % 2]
                act(s.t[:, 0:N], src[:, kc, 0:N], AF.Square, [srcb[kc]], [s])
                mm(p.t[:, 0:N], onesB.t[:], s.t[:, 0:N], kc == 0, kc == KC - 1, [onesB, s], [p])
            ts("dve", rstd.t[:, 0:N], p.t[:, 0:N], 1.0 / D, EPS, ALU.mult, ALU.add, [p], [rstd])
            act(rstd.t[:, 0:N], rstd.t[:, 0:N], AF.Sqrt, [rstd], [rstd])
            recip(rstd.t[:, 0:N], rstd.t[:, 0:N], [rstd], [rstd])

        def ada_in(src, srcb, j, segs):
            for kc in range(KC):
                for (lo, hi, mi) in segs:
                    tm = tmpf[kc % 2]
                    tt("dve", tm.t[:, lo:hi], src[:, kc, lo:hi], rstd.t[:, lo:hi], ALU.mult, [srcb[kc], rstd], [tm])
                    act(yT.t[:, kc, lo:hi], tm.t[:, lo:hi], AF.Identity, [tm, AIN, MODT], [yT.b[kc]],
                        bias=bin_(j, kc, mi), scale=ain(j, kc, mi))

        def ada_out(j, segs):
            for kc in range(KC):
                for (lo, hi, mi) in segs:
                    tm = tmpf[kc % 2]
                    tt("dve", tm.t[:, lo:hi], zT[:, kc, lo:hi], rstd.t[:, lo:hi], ALU.mult, [zTb[kc], rstd], [tm])
                    stt("dve", xT[:, kc, lo:hi], tm.t[:, lo:hi], aout(j, kc, mi), xT[:, kc, lo:hi], ALU.mult, ALU.add,
                        [tm, AOUT, xTb[kc]], [xTb[kc]])

        wctr = [0, 0]

        def ffn(f, N):
            for j in range(JC):
                wb = WIN[wctr[0] % 3]
                wctr[0] += 1
                dma(wb.t[:].rearrange("p k c -> p (k c)"), Win_s[f * JC + j], [dB["Win"][f * JC + j]], [wb],
                    f"win{(wctr[0] - 1) % 3}")
                pa, pb = nps(), nps()
                for kc in range(KC):
                    mm(pa.t[:, 0:N], wb.t[:, kc, 0:128], yT.t[:, kc, 0:N], kc == 0, kc == KC - 1, [wb, yT.b[kc]], [pa])
                for kc in range(KC):
                    mm(pb.t[:, 0:N], wb.t[:, kc, 128:256], yT.t[:, kc, 0:N], kc == 0, kc == KC - 1, [wb, yT.b[kc]], [pb])
                sa = tmpf[2]
                act(sa.t[:, 0:N], pa.t[:, 0:N], AF.Silu, [pa], [sa])
                tt("dve", hTv[:, j, 0:N], sa.t[:, 0:N], pb.t[:, 0:N], ALU.mult, [sa, pb], [hT.b[j]])
            for m in range(KC):
                wo = WOUT[wctr[1] % 2]
                wctr[1] += 1
                dma(wo.t[:].rearrange("p j c -> p (j c)"), Wout_s[f * KC + m], [dB["Wout"][f * KC + m]], [wo],
                    f"wout{(wctr[1] - 1) % 2}")
                pz = nps()
                for j in range(JC):
                    mm(pz.t[:, 0:N], wo.t[:, j, :], hTv[:, j, 0:N], j == 0, j == JC - 1, [wo, hT.b[j]], [pz])
                cp("act", zT[:, m, 0:N], pz.t[:, 0:N], [pz], [zTb[m]])

        QK0, V0, O0, G0, P0 = 0, 512, 1024, 1536, 1552
        wiv = Wi_s.ap().rearrange("p (kc c) -> p kc c", kc=KC)

        def inproj_fm(pq, N, off):
            for h in range(4):
                wb = WIN[wctr[0] % 3]
                wctr[0] += 1
                dma(wb.t[:, :, 0:128], wiv[:, :, QK0 + h * 128:QK0 + (h + 1) * 128], [dB["Wi"][0]], [wb],
                    f"win{(wctr[0] - 1) % 3}")
                p = nps()
                for kc in range(KC):
                    mm(p.t[:, 0:N], wb.t[:, kc, 0:128], yT.t[:, kc, 0:N], kc == 0, kc == KC - 1, [wb, yT.b[kc]], [p])
                cp("act", pq.t[:, h, off:off + N], p.t[:, 0:N], [p], [pq])

        gp_cnt = [0]

        def gate_pack(ci, lhs_chunk_ap, lhs_bufs):
            p = nps()
            for kc in range(KC):
                mm(p.t[:, 0:16], lhs_chunk_ap(kc), WG.t[:, kc, :], kc == 0, kc == KC - 1, [WG] + lhs_bufs, [p])
            tt("dve", GA.t[:], p.t[:, 0:16], GBIAS.t[:], ALU.add, [p, GBIAS], [GA])
            act(LF.t[:], GA.t[:, 8:16], AF.Exp, [GA], [LF], scale=-1.0)
            act(LF.t[:], LF.t[:], AF.Ln, [LF], [LF], bias=1.0)
            ts1("dve", LF.t[:], LF.t[:], -1.0, ALU.mult, [LF], [LF])
            p2 = nps()
            mm(p2.t[:, 0:4], triF.t[:], LF.t[:, 0:4], True, True, [triF, LF], [p2])
            mm(p2.t[:, 4:8], triB.t[:], LF.t[:, 4:8], True, True, [triB, LF], [p2])
            mm(p2.t[:, 8:16], onesF.t[:], LF.t[:], True, True, [onesF, LF], [p2])
            gb = [GP.b[ci]]
            tt("dve", G8.t[:], GA.t[:, 0:8], p2.t[:, 0:8], ALU.subtract, [GA, p2], [G8])
            act(GP.t[:, ci, 0:8], G8.t[:], AF.Exp, [G8], gb)
            act(GP.t[:, ci, 8:16], p2.t[:, 0:8], AF.Exp, [p2], gb)
            act(GP.t[:, ci, 16:24], p2.t[:, 8:16], AF.Exp, [p2], gb)
            cp("dve", GP.t[:, ci, 24:32], p2.t[:, 8:16], [p2], gb)

        def inproj_tm(N, nchunks, chunk0, vx_list, full, gblk0):
            dma(WG.t[:], wiv[:, :, G0:G0 + 16], [dB["Wi"][0]], [WG], "wg")
            for ci in range(nchunks):
                gate_pack(chunk0 + ci, lambda kc, ci=ci: yT.t[:, kc, ci * 128:(ci + 1) * 128], list(yT.b))
            pieces = [("v", V0)] + ([("o", O0), ("p", P0)] if full else [])
            if 'op' in KOFF:
                pieces = pieces[0:1]
            if 'p' in KOFF:
                pieces = pieces[0:2]
            for nm, c0 in pieces:
                wb = WPC[wctr[0] % 2]
                wctr[0] += 1
                dma(wb.t[:], wiv[:, :, c0:c0 + 512], [dB["Wi"][0]], [wb], f"wpc{(wctr[0] - 1) % 2}")
                for ci in range(nchunks):
                    p = nps()
                    for kc in range(KC):
                        mm(p.t[:], yT.t[:, kc, ci * 128:(ci + 1) * 128], wb.t[:, kc, :], kc == 0, kc == KC - 1,
                           [wb, yT.b[kc]], [p])
                    gb = gblk0 + ci
                    if nm == "v":
                        vx = vx_list[ci]
                        cp("act", vx.t[:, :, 0:128], p.t[:].rearrange("p (h c) -> p h c", h=4), [p], [vx])
                        if full and 'v' not in KOFF:
                            dma(V_s[gb], vx.t[:].rearrange("p h c -> p (h c)"), [vx], [dB["V"][gb]], f"vs{ci % 2}")
                    elif nm == "o":
                        o = OT[ci % 2]
                        cp("act", o.t[:], p.t[:], [p], [o])
                        if 'o' not in KOFF:
                            dma(O_s[gb], o.t[:], [o], [dB["O"][gb]], f"os{ci % 2}")
                    else:
                        pt, ptb = PT[ci % 2], PTb[ci % 2]
                        if 'pa' not in KOFF:
                            cp("act", pt.t[:], p.t[:], [p], [pt])
                        if 'pb' not in KOFF:
                            cp("act", ptb.t[:], p.t[:], [p], [ptb])
                        if 'pl' not in KOFF:
                            dma(PL_s[gb], pt.t[:], [pt], [dB["PL"][gb]], f"pls{ci % 2}")
                            dma(PLb_s[gb], ptb.t[:], [ptb], [dB["PLb"][gb]], f"plb{ci % 2}")
                        if gb < 4 and 'cc2' not in KOFF:
                            dma(cc2_in[:, gb * 512:(gb + 1) * 512], ptb.t[:], [ptb], [dB["cc2i"][0]], f"plc{ci % 2}")
                        if gb >= NB - 4 and 'cc2' not in KOFF:
                            k = 4 + gb - (NB - 4)
                            dma(cc2_in[:, k * 512:(k + 1) * 512], ptb.t[:], [ptb], [dB["cc2i"][0]], f"pld{ci % 2}")

        def conv_u(pq, N):
            for h in range(4):
                tm = tmpf[h % 2]
                ts("dve", tm.t[:, 0:N], pq.t[:, h, 0:N], vcol("cw", 0 * 4 + h), vcol("cb", h), ALU.mult, ALU.add,
                   [pq, VT], [tm])
                stt("dve", tm.t[:, 0:N], pq.t[:, h, 1:N + 1], vcol("cw", 1 * 4 + h), tm.t[:, 0:N], ALU.mult, ALU.add,
                    [pq, VT, tm], [tm])
                stt("dve", tm.t[:, 0:N], pq.t[:, h, 2:N + 2], vcol("cw", 2 * 4 + h), tm.t[:, 0:N], ALU.mult, ALU.add,
                    [pq, VT, tm], [tm])
                act(uT.t[:, h, 0:N], tm.t[:, 0:N], AF.Silu, [tm], [uT])

        KSC = 128.0 ** -0.5

        def ktok_chunk(ci_local, kt):
            p = nps()
            for h in range(4):
                mm(p.t[:, h * 128:(h + 1) * 128], uT.t[:, h, ci_local * 128:(ci_local + 1) * 128], wk.t[:, h, :], True, True,
                   [uT, wk], [p])
            act(kt.t[:].rearrange("p h c -> p (h c)"), p.t[:], AF.Copy, [p], [kt], scale=KSC)

        def ckv(kt, vw, pair):
            p = nps()
            pv = p.t[:, 0:258].rearrange("p (h c) -> p h c", h=2)
            for hh in range(2):
                h = pair * 2 + hh
                mm(pv[:, hh, :], kt.t[:, h, :], vw.t[:, h, :], True, True, [kt, vw], [p])
            return p, pv

        def vw_make(vw, vx, ci, d):
            tt("dve", vw.t[:], vx.t[:], GP.t[:, ci, d * 4:(d + 1) * 4].unsqueeze(2).to_broadcast([128, 4, 129]), ALU.mult,
               [vx, GP.b[ci]], [vw])

        def state_step_A(ci, kt, vx, snap_gb):
            if snap_gb is not None and 'snap' not in KOFF:
                sn = SNP[snap_gb % 2]
                cp("act", sn.t[:], C_b.t[:], [C_b], [sn])
                dma(SNAP_s[snap_gb], sn.t[:].rearrange("p h c -> p (h c)"), [sn], [dB["SNAP"][snap_gb]], f"sn{snap_gb % 2}")
                cp("pool", GPX.t[:, ci, :], ESB.t[:], [ESB], [GPX.b[ci]])
                tt("pool", WCB.t[:], ESB.t[:], GP.t[:, ci, 20:24], ALU.mult, [ESB, GP.b[ci]], [WCB])
                cp("pool", ESB.t[:], WCB.t[:], [WCB], [ESB])
            vw = VW[0]
            vw_make(vw, vx, ci, 1)
            if KSTOP <= 3.86:
                return
            for pair in range(2):
                p, pv = ckv(kt, vw, pair)
                if KSTOP <= 3.87:
                    return
                rt = RT[pair]
                tt("dve", rt.t[:], pv, C_b.t[:, pair * 2:pair * 2 + 2, :], ALU.add, [p, C_b], [rt])
                tt("dve", C_b.t[:, pair * 2:pair * 2 + 2, :], rt.t[:],
                   GP.t[:, ci, 20 + pair * 2:22 + pair * 2].unsqueeze(2).to_broadcast([128, 2, 129]), ALU.mult,
                   [rt, GP.b[ci]], [C_b])
            if KSTOP <= 3.88:
                return
            vw = VW[1]
            vw_make(vw, vx, ci, 0)
            tt("pool", WC.t[:], ESF.t[:], GP.t[:, ci, 16:20], ALU.mult, [ESF, GP.b[ci]], [WC])
            cp("pool", ESF.t[:], WC.t[:], [WC], [ESF])
            if KSTOP <= 3.89:
                return
            for pair in range(2):
                p, pv = ckv(kt, vw, pair)
                rt = RT[pair]
                tt("dve", rt.t[:], pv, WC.t[:, pair * 2:pair * 2 + 2].unsqueeze(2).to_broadcast([128, 2, 129]), ALU.mult,
                   [p, WC], [rt])
                tt("dve", T_f.t[:, pair * 2:pair * 2 + 2, :], T_f.t[:, pair * 2:pair * 2 + 2, :], rt.t[:], ALU.add,
                   [T_f, rt], [T_f])

        SEG_MAIN = [(0, 512, 0)]
        SEG_AUX = [(0, cfg.CTX, 1), (cfg.CTX, NA, 0)]

        for t_ in (C_b, T_f, GT):
            memset("pool", t_.t[:], 0.0, [t_])
        memset("pool", ESF.t[:], 1.0, [ESF]); memset("pool", ESB.t[:], 1.0, [ESB])
        load_xT(x_aux.ap(), NA)
        if KSTOP <= 3.1:
            P.emit()
            return nc
        stats(xT, xTb, NA)
        if KSTOP <= 3.2:
            P.emit()
            return nc
        ada_in(xT, xTb, 0, SEG_AUX)
        if KSTOP <= 3.3:
            P.emit()
            return nc
        ffn(0, NA)
        if KSTOP <= 3.4:
            P.emit()
            return nc
        stats(zT, zTb, NA)
        ada_out(0, SEG_AUX)
        stats(xT, xTb, NA)
        ada_in(xT, xTb, 1, SEG_AUX)
        if KSTOP <= 3.5:
            P.emit()
            return nc
        pqa = PQ[1]
        memset("pool", pqa.t[:], 0.0, [pqa])
        inproj_fm(pqa, NA, 1)
        if KSTOP <= 3.6:
            P.emit()
            return nc
        ts1("dve", HL.t[:], pqa.t[:, :, 1 + cfg.CTX], flags.t[:, 2:3], ALU.mult, [pqa, flags], [HL])
        ts1("dve", HR.t[:], pqa.t[:, :, 2 + cfg.CTX], flags.t[:, 3:4], ALU.mult, [pqa, flags], [HR])
        memset("dve", pqa.t[:, :, 1 + cfg.CTX:2 + cfg.CTX], 0.0, [pqa])
        inproj_tm(NA, CC, NB, VXT[0:CC], False, 0)
        if KSTOP <= 3.7:
            P.emit()
            return nc
        conv_u(pqa, cfg.CTX)
        if KSTOP <= 3.8:
            P.emit()
            return nc
        for ci in reversed(range(CC)):
            kt = KTOK[ci % 2]
            ktok_chunk(ci, kt)
            if KSTOP <= 3.85:
                P.emit()
                return nc
            state_step_A(NB + ci, kt, VXT[ci], None)
            if KSTOP <= 3.9:
                P.emit()
                return nc
        cp("pool", FCTX.t[:], T_f.t[:], [T_f], [FCTX])
        memset("pool", T_f.t[:], 0.0, [T_f])
        ts1("dve", C_b.t[:], C_b.t[:], flags.t[:, 1:2], ALU.mult, [C_b, flags], [C_b])
        memset("pool", ESF.t[:], 1.0, [ESF]); memset("pool", ESB.t[:], 1.0, [ESB])

        if KSTOP <= 4:
            P.emit()
            return nc
        def prep_tile(i):
            pq = PQ[i % 2]
            conv_u(pq, 512)
            for h in range(4):
                p = nps()
                mm(p.t[:], wq.t[:, h, :], uT.t[:, h, :], True, True, [wq, uT], [p])
                cp("act", QT.t[:, h, :], p.t[:], [p], [QT])
                p = nps()
                mm(p.t[:], wk.t[:, h, :], uT.t[:, h, :], True, True, [wk, uT], [p])
                act(KT.t[:, h, :], p.t[:], AF.Copy, [p], [KT], scale=KSC)
            dma(QT_s[i], QT.t[:].rearrange("p h n -> p (h n)"), [QT], [dB["QT"][i]], "qts")
            dma(KT_s[i], KT.t[:].rearrange("p h n -> p (h n)"), [KT], [dB["KT"][i]], "kts")
            if KSTOP <= 4.4:
                return
            for cl in reversed(range(4)):
                gb = i * 4 + cl
                kt = KTOK[cl % 2]
                ktok_chunk(cl, kt)
                dma(KTOK_s[gb], kt.t[:].rearrange("p h c -> p (h c)"), [kt], [dB["KTOK"][gb]], f"kk{cl % 2}")
                state_step_A(gb, kt, VXT[(i % 2) * 4 + cl], gb)
                if 'gt' not in KOFF:
                    tt("pool", GT.t[:], GT.t[:], GP.t[:, gb, 24:32], ALU.add, [GT, GP.b[gb]], [GT])

        for step in range(NT + 1):
            i = NT - 1 - step
            if i >= 0:
                load_xT(x_loc[i * 512:(i + 1) * 512, :], 512)
                stats(xT, xTb, 512)
                ada_in(xT, xTb, 0, SEG_MAIN)
                ffn(0, 512)
                stats(zT, zTb, 512)
                ada_out(0, SEG_MAIN)
                dma(X1_s[i], BIG1.t[:, 0:KC * 512], xTb, [dB["X1"][i]], "x1s")
                if KSTOP <= 4.1:
                    P.emit()
                    return nc
                stats(xT, xTb, 512)
                ada_in(xT, xTb, 1, SEG_MAIN)
                pq = PQ[i % 2]
                inproj_fm(pq, 512, 1)
                if i == NT - 1:
                    cp("pool", pq.t[:, :, 513], HR.t[:], [HR], [pq])
                else:
                    pqn = PQ[(i + 1) % 2]
                    cp("pool", pq.t[:, :, 513], pqn.t[:, :, 1], [pqn], [pq])
                    cp("pool", pqn.t[:, :, 0], pq.t[:, :, 512], [pq], [pqn])
                if i == 0:
                    cp("pool", pq.t[:, :, 0], HL.t[:], [HL], [pq])
                if KSTOP <= 4.2:
                    P.emit()
                    return nc
                inproj_tm(512, 4, i * 4, VXT[(i % 2) * 4:(i % 2) * 4 + 4], True, i * 4)
                if KSTOP <= 4.3:
                    P.emit()
                    return nc
            if i + 1 <= NT - 1:
                prep_tile(i + 1)
        if KSTOP <= 5:
            P.emit()
            return nc
        tt("dve", C_f.t[:], FCTX.t[:], ESF.t[:].unsqueeze(2).to_broadcast([128, 4, 129]), ALU.mult, [FCTX, ESF], [C_f])
        stt("dve", T_f.t[:], C_f.t[:], flags.t[:, 0:1], T_f.t[:], ALU.mult, ALU.add, [C_f, flags, T_f], [T_f])
        dma(cc1_in[:, 0:516], T_f.t[:].rearrange("p h c -> p (h c)"), [T_f], [dB["cc1i"][0]], "cc1w")
        dma(cc1_in[:, 516:1032], C_b.t[:].rearrange("p h c -> p (h c)"), [C_b], [dB["cc1i"][0]], "cc1w", True)
        dma(cc1_in[:, 1032:1040], GT.t[:], [GT], [dB["cc1i"][0]], "cc1w", True)
        P.cc(lambda e: e.collective_compute("AllGather", ALU.bypass, replica_groups=[list(range(NCORES))],
                                            ins=[cc1_in.ap().opt()], outs=[cc1_out.ap().opt()]),
             bl(dB["cc1i"]), bl(dB["cc1o"]), sem="cc1")
        P.cc(lambda e: e.collective_compute("AllGather", ALU.bypass, replica_groups=[list(range(NCORES))],
                                            ins=[cc2_in.ap().opt()], outs=[cc2_out.ap().opt()]),
             bl(dB["cc2i"]), bl(dB["cc2o"]), sem="cc2")

        if KSTOP <= 6:
            P.emit()
            return nc
        g1 = cc1_out.ap().rearrange("(q p) c -> p q c", p=128)
        dma(GTQ.t[:], g1[:, :, 1032:1040], [dB["cc1o"][0]], [GTQ], "gtq")
        cp("dve", GTT.t[:], GTQ.t[:].rearrange("p q k -> p k q"), [GTQ], [GTT])
        for d in range(2):
            for q in range(8):
                tt("dve", TM48.t[:], GTT.t[:, d * 4:(d + 1) * 4, :], mbt.t[:, d, q, :].unsqueeze(1).to_broadcast([128, 4, 8]),
                   ALU.mult, [GTT, mbt], [TM48])
                P.op("dve", lambda e, q=q, d=d: e.tensor_reduce(ARGS.t[:, q, d * 4:(d + 1) * 4], TM48.t[:], AX.X, ALU.add),
                     bl(TM48), bl(ARGS))
        act(WQ8.t[:], ARGS.t[:], AF.Exp, [ARGS], [WQ8])
        tt("dve", WQ8.t[:], WQ8.t[:], predt.t[:], ALU.mult, [WQ8, predt], [WQ8])
        ts1("dve", C_f.t[:], FCTX.t[:], flags.t[:, 0:1], ALU.mult, [FCTX, flags], [C_f])
        memset("pool", SINB.t[:], 0.0, [SINB])
        for q in range(8):
            s_ = STR[q % 2]
            dma(s_.t[:], cc1_out[q * 128:(q + 1) * 128, :], [dB["cc1o"][0]], [s_], f"str{q % 2}")
            for d, dst in ((0, C_f), (1, SINB)):
                sv = s_.t[:, d * 516:(d + 1) * 516].rearrange("p (h c) -> p h c", h=4)
                tt("dve", TMPS.t[:], sv, WQ8.t[:, q, d * 4:(d + 1) * 4].unsqueeze(2).to_broadcast([128, 4, 129]), ALU.mult,
                   [s_, WQ8], [TMPS])
                tt("dve", dst.t[:], dst.t[:], TMPS.t[:], ALU.add, [dst, TMPS], [dst])
        memset("pool", HALO.t[:], 0.0, [HALO])
        for q in range(8):
            hq = HQ[q % 2]
            dma(hq.t[:].rearrange("p a b -> p (a b)"), cc2_out[q * 128:(q + 1) * 128, :], [dB["cc2o"][0]], [hq], f"hq{q % 2}")
            stt("dve", HALO.t[:, 0:4, :], hq.t[:, 4:8, :], sel.t[:, 0, q:q + 1], HALO.t[:, 0:4, :], ALU.mult, ALU.add,
                [hq, sel, HALO], [HALO])
            stt("dve", HALO.t[:, 4:8, :], hq.t[:, 0:4, :], sel.t[:, 1, q:q + 1], HALO.t[:, 4:8, :], ALU.mult, ALU.add,
                [hq, sel, HALO], [HALO])

        if KSTOP <= 7:
            P.emit()
            return nc
        og = {g: [(idx, dl) for idx, (gg, dl) in enumerate(offs) if gg == g] for g in range(4)}
        ring_loaded = [-1]
        hsv = HS.t[:]
        hnv = HNORM.t[:].rearrange("p (h c) -> p h c", h=4)

        def mixer(gb, cl):
            ci = gb
            kt, vx, ot, sn = KTOK[cl % 2], VXT[cl % 2], OT[cl % 2], SNP[cl % 2]
            cs = slice(cl * 128, (cl + 1) * 128)
            p = nps()
            pk = p.t[:].rearrange("p (h c) -> p h c", h=4)
            for h in range(4):
                mm(pk[:, h, :], KT.t[:, h, cs], QT.t[:, h, cs], True, True, [KT, QT], [p])
            tt("dve", SM[0].t[:], pk, triF.t[:].unsqueeze(1).to_broadcast([128, 4, 128]), ALU.mult, [p, triF], [SM[0]])
            tt("dve", SM[1].t[:], pk, triB.t[:].unsqueeze(1).to_broadcast([128, 4, 128]), ALU.mult, [p, triB], [SM[1]])
            cp("act", CFB.t[:], C_f.t[:], [C_f], [CFB])
            tt("pool", CBC.t[:], SINB.t[:], GPX.t[:, ci, :].unsqueeze(2).to_broadcast([128, 4, 129]), ALU.mult,
               [SINB, GPX.b[ci]], [CBC])
            for d in range(2):
                vw_make(VW[d], vx, ci, d)
            for d in range(2):
                vw = VW[d]
                for pair in range(2):
                    p = nps()
                    pv = p.t[:, 0:258].rearrange("p (h c) -> p h c", h=2)
                    for hh in range(2):
                        h = pair * 2 + hh
                        mm(pv[:, hh, :], SM[d].t[:, h, :], vw.t[:, h, :], True, False, [SM[d], vw], [p])
                        if d == 0:
                            mm(pv[:, hh, :], QT.t[:, h, cs], CFB.t[:, h, :], False, True, [QT, CFB], [p])
                        else:
                            mm(pv[:, hh, :], QT.t[:, h, cs], sn.t[:, h, :], False, False, [QT, sn], [p])
                            mm(pv[:, hh, :], QT.t[:, h, cs], CBC.t[:, h, :], False, True, [QT, CBC], [p])
                    nd = ND[pair]
                    e0 = 8 + d * 4 + pair * 2
                    tt("dve", nd.t[:], pv, GP.t[:, ci, e0:e0 + 2].unsqueeze(2).to_broadcast([128, 2, 129]), ALU.mult,
                       [p, GP.b[ci]], [nd])
                    a8, r8 = SM8[0], SM8[1]
                    act(a8.t[:, 0:2], nd.t[:, :, 128], AF.Abs, [nd], [a8])
                    ts1("dve", a8.t[:, 0:2], a8.t[:, 0:2], 1.0, ALU.max, [a8], [a8])
                    recip(r8.t[:, 0:2], a8.t[:, 0:2], [a8], [r8])
                    for hh in range(2):
                        h = pair * 2 + hh
                        if d == 0:
                            ts1("dve", HS.t[:, h, :], nd.t[:, hh, 0:128], r8.t[:, hh:hh + 1], ALU.mult, [nd, r8], [HS])
                        else:
                            stt("dve", HS.t[:, h, :], nd.t[:, hh, 0:128], r8.t[:, hh:hh + 1], HS.t[:, h, :], ALU.mult, ALU.add,
                                [nd, r8, HS], [HS])
            for pair in range(2):
                p, pv = ckv(kt, VW[0], pair)
                rt = RT[pair]
                tt("dve", rt.t[:], pv, C_f.t[:, pair * 2:pair * 2 + 2, :], ALU.add, [p, C_f], [rt])
                tt("dve", C_f.t[:, pair * 2:pair * 2 + 2, :], rt.t[:],
                   GP.t[:, ci, 16 + pair * 2:18 + pair * 2].unsqueeze(2).to_broadcast([128, 2, 129]), ALU.mult,
                   [rt, GP.b[ci]], [C_f])
            tt("pool", HSQ.t[:], HS.t[:], HS.t[:], ALU.mult, [HS], [HSQ])
            s2, s3 = SM8[2], SM8[3]
            P.op("dve", lambda e: e.tensor_reduce(s2.t[:, 0:4], HSQ.t[:], AX.X, ALU.add), bl(HSQ), bl(s2))
            ts("dve", s2.t[:, 0:4], s2.t[:, 0:4], 1.0 / 128, EPS, ALU.mult, ALU.add, [s2], [s2])
            act(s2.t[:, 0:4], s2.t[:, 0:4], AF.Sqrt, [s2], [s2])
            recip(s3.t[:, 0:4], s2.t[:, 0:4], [s2], [s3])
            tt("dve", HS.t[:], HS.t[:], s3.t[:, 0:4].unsqueeze(2).to_broadcast([128, 4, 128]), ALU.mult, [HS, s3], [HS])
            tt("dve", HS.t[:], HS.t[:], hnv, ALU.mult, [HS, HNORM], [HS])
            act(SG.t[:], ot.t[:], AF.Sigmoid, [ot], [SG])
            tt("dve", MO.t[:], HS.t[:], SG.t[:].rearrange("p (h c) -> p h c", h=4), ALU.mult, [HS, SG], [MO])
            for h in range(4):
                tr(PSB.t[:, h, :], MO.t[:, h, :], identB.t[:], [MO, identB], [PSB])
            cp("act", MIXT.t[:, 0:4, cs], PSB.t[:, 0:4, :], [PSB], MIXT.b[0:4])

        def pool_chunk(gb, cl):
            cs = slice(cl * 128, (cl + 1) * 128)
            for blk in range(ring_loaded[0] + 1, min(gb + 4, NB - 1) + 1):
                rg = RING[blk % 10]
                dma(rg.t[:], PLb_s[blk], [dB["PLb"][blk]], [rg], f"rg{blk % 10}")
                ring_loaded[0] = blk
            p = nps()
            pv4 = p.t[:].rearrange("p (g c) -> p g c", g=4)
            for g in range(4):
                lst = og[g]
                for n, (idx, dl) in enumerate(lst):
                    blk = gb + dl
                    if blk < 0:
                        src, sbuf = HALO.t[:, 4 + blk, g * 128:(g + 1) * 128], HALO
                    elif blk >= NB:
                        src, sbuf = HALO.t[:, 4 + blk - NB, g * 128:(g + 1) * 128], HALO
                    else:
                        src, sbuf = RING[blk % 10].t[:, g * 128:(g + 1) * 128], RING[blk % 10]
                    mm(pv4[:, g, :], PM.t[:, idx, :], src, n == 0, n == len(lst) - 1, [PM, sbuf], [p])
            tm = tmpf[0]
            tmv = tm.t[:].rearrange("p (g c) -> p g c", g=4)
            tt("dve", tmv, pv4, invcnt.t[:, gb, :].unsqueeze(2).to_broadcast([128, 4, 128]), ALU.mult, [p, invcnt], [tm])
            pt = PT[cl % 2]
            tt("dve", DT_.t[:], tmv, pt.t[:].rearrange("p (g c) -> p g c", g=4), ALU.subtract, [tm, pt], [DT_])
            for g in range(4):
                tr(PSB.t[:, 4 + g, :], DT_.t[:, g, :], identB.t[:], [DT_, identB], [PSB])
            cp("act", DTT.t[:], PSB.t[:, 4:8, :], [PSB], [DTT])
            for g in range(4):
                p = nps()
                mm(p.t[:, 0:128], pw.t[:, g, :], DTT.t[:, g, :], True, True, [pw, DTT], [p])
                act(MIXT.t[:, 4 + g, cs], p.t[:, 0:128], AF.Copy, [p, VT], [MIXT.b[4 + g]], scale=vcol("psc", g))

        wov_src = Wo_s.ap().rearrange("p (mc c) -> p mc c", mc=8)
        for i in range(NT):
            ring_loaded[0] = max(i * 4 - 5, -1)
            dma(BIG1.t[:, 0:KC * 512], X1_s[i], [dB["X1"][i]], xTb, "x1l")
            dma(QT.t[:].rearrange("p h n -> p (h n)"), QT_s[i], [dB["QT"][i]], [QT], "qtl")
            dma(KT.t[:].rearrange("p h n -> p (h n)"), KT_s[i], [dB["KT"][i]], [KT], "ktl")
            for cl in range(4):
                gb = i * 4 + cl
                dma(KTOK[cl % 2].t[:].rearrange("p h c -> p (h c)"), KTOK_s[gb], [dB["KTOK"][gb]], [KTOK[cl % 2]], f"kkl{cl % 2}")
                dma(VXT[cl % 2].t[:].rearrange("p h c -> p (h c)"), V_s[gb], [dB["V"][gb]], [VXT[cl % 2]], f"vl{cl % 2}")
                dma(OT[cl % 2].t[:], O_s[gb], [dB["O"][gb]], [OT[cl % 2]], f"ol{cl % 2}")
                dma(SNP[cl % 2].t[:].rearrange("p h c -> p (h c)"), SNAP_s[gb], [dB["SNAP"][gb]], [SNP[cl % 2]], f"snl{cl % 2}")
                dma(PT[cl % 2].t[:], PL_s[gb], [dB["PL"][gb]], [PT[cl % 2]], f"pll{cl % 2}")
                mixer(gb, cl)
                pool_chunk(gb, cl)
            for m in range(KC):
                wb = WPC[wctr[0] % 2]
                wctr[0] += 1
                wbv = wb.t[:].rearrange("p k c -> p (k c)")[:, 0:1024].rearrange("p (mc c) -> p mc c", mc=8)
                dma(wbv, wov_src[:, :, m * 128:(m + 1) * 128], [dB["Wo"][0]], [wb], f"wpc{(wctr[0] - 1) % 2}")
                pz = nps()
                for mc in range(8):
                    mm(pz.t[:], wbv[:, mc, :], MIXT.t[:, mc, :], mc == 0, mc == 7, [wb, MIXT.b[mc]], [pz])
                cp("act", zT[:, m, :], pz.t[:], [pz], [zTb[m]])
            stats(zT, zTb, 512)
            ada_out(1, SEG_MAIN)
            stats(xT, xTb, 512)
            ada_in(xT, xTb, 2, SEG_MAIN)
            ffn(1, 512)
            stats(zT, zTb, 512)
            ada_out(2, SEG_MAIN)
            for bi in range(4):
                xo = XIO[bi % 2]
                for k0 in range(0, KC, 4):
                    kn = min(4, KC - k0)
                    p = nps()
                    for k in range(kn):
                        tr(p.t[:, k * 128:(k + 1) * 128], xT[:, k0 + k, bi * 128:(bi + 1) * 128], identF.t[:],
                           [xTb[k0 + k], identF], [p])
                    cp("act", xo.t[:, k0 * 128:(k0 + kn) * 128], p.t[:, 0:kn * 128], [p], [xo])
                r0_ = i * 512 + bi * 128
                dma(out_d[r0_:r0_ + 128, :], xo.t[:], [xo], [dB["out"][0]], f"xin{bi % 2}")
        P.op("sp", None, bl(dB["out"]), [])
        P.emit()
    return nc


_NC_CACHE = {}


def make_in_maps(cfg, inp):
    NT = cfg.NT
    x = np.ascontiguousarray(inp["x"], dtype=np.float32)
    ctx = np.asarray(inp["ctx"], np.float32)
    maps = []
    shared = {
        "c_ctx": np.ascontiguousarray(inp["c_ctx"], np.float32).reshape(-1),
        "w_mod": np.ascontiguousarray(inp["w_mod"][0], np.float32),
        "b_mod": np.ascontiguousarray(inp["b_mod"][0], np.float32).reshape(-1),
        "norm_pre": np.ascontiguousarray(inp["norm_pre"][0], np.float32).reshape(-1),
        "norm_post": np.ascontiguousarray(inp["norm_post"][0], np.float32).reshape(-1),
        "ffn_w_in": np.ascontiguousarray(inp["ffn_w_in"][0], np.float32),
        "ffn_w_out": np.ascontiguousarray(inp["ffn_w_out"][0], np.float32),
        "w_in": np.ascontiguousarray(inp["w_in"][0], np.float32),
        "w_out": np.ascontiguousarray(inp["w_out"][0], np.float32),
        "conv_w": np.ascontiguousarray(inp["conv_w"][0], np.float32).reshape(-1),
        "conv_b": np.ascontiguousarray(inp["conv_b"][0], np.float32).reshape(-1),
        "w_q": np.ascontiguousarray(inp["w_q"][0], np.float32),
        "w_k": np.ascontiguousarray(inp["w_k"][0], np.float32),
        "gbias": np.concatenate([np.asarray(inp["i_bias"][0], np.float32).reshape(-1),
                                 np.asarray(inp["f_bias"][0], np.float32).reshape(-1)]),
        "head_norm": np.ascontiguousarray(inp["head_norm"][0], np.float32).reshape(-1),
        "pool_w": np.ascontiguousarray(inp["pool_w"][0], np.float32),
        "pool_scale": np.ascontiguousarray(inp["pool_scale"][0], np.float32).reshape(-1),
    }
    for core in range(NCORES):
        b, r = core // 4, core % 4
        t0 = r * NT * 512
        m = dict(shared)
        m["x_loc"] = np.ascontiguousarray(x[b, t0:t0 + NT * 512])
        aux = np.zeros((cfg.NA, cfg.D), np.float32)
        aux[0:cfg.CTX] = ctx[b]
        if r > 0:
            aux[cfg.CTX] = x[b, t0 - 1]
        if r < 3:
            aux[cfg.CTX + 1] = x[b, t0 + NT * 512]
        m["x_aux"] = aux
        m["c_loc"] = np.ascontiguousarray(inp["c"][b], np.float32).reshape(-1)
        m.update(host_tables(cfg, core))
        maps.append(m)
    return maps


def run_cfg(cfg, inp):
    key = (cfg.D, cfg.DFF, cfg.NT, cfg.CTX)
    if key not in _NC_CACHE:
        _NC_CACHE[key] = build(cfg)
    nc = _NC_CACHE[key]
    maps = make_in_maps(cfg, inp)
    res = run_bass_kernel_spmd(nc, maps, core_ids=list(range(NCORES)))
    NT = cfg.NT
    out = np.zeros((2, 4 * NT * 512, cfg.D), np.float32)
    for core in range(NCORES):
        b, r = core // 4, core % 4
        out[b, r * NT * 512:(r + 1) * NT * 512] = res.results[core]["out"]
    return out


def kernel(**inputs):
    cfg = Cfg()
    return run_cfg(cfg, inputs)
```
